# Optimizing a Trainium2 kernel written in Bass

```python
import jax, jax.numpy as jnp
from jax import lax
import numpy as np

D_MODEL = 2048
BATCH = 4
SEQ = 8192
DEPTH = 1

N_HEADS = 8
N_KV_HEADS = 2
HEAD_DIM = 128
ATTN_WIDTH = N_HEADS * HEAD_DIM
KV_WIDTH = N_KV_HEADS * HEAD_DIM
ROPE_THETA = 500000.0
ROPE_FRACTION = 4
IDX_HEADS = 16
IDX_DIM = 64
TOPK_MAX = 256
Q_BLOCK = 128
POOL_GROUPS = 4
POOL_WINDOWS = (2, 4, 8, 16)
POOL_WIDTH = D_MODEL - ATTN_WIDTH
POOL_GROUP_DIM = POOL_WIDTH // POOL_GROUPS
MIX_WIDTH = ATTN_WIDTH + POOL_WIDTH
N_EXPERT_GROUPS = 4
EXPERTS_PER_GROUP = 8
N_EXPERTS = N_EXPERT_GROUPS * EXPERTS_PER_GROUP
EXPERT_TOPK = 2
D_EXPERT = 512
NORM_EPS = 1e-6
PROJ_SPLITS = (ATTN_WIDTH, KV_WIDTH, KV_WIDTH, IDX_HEADS * IDX_DIM, IDX_DIM, IDX_HEADS, POOL_WIDTH)
PROJ_WIDTH = sum(PROJ_SPLITS)

kernel_name = "hymba_dsa_pool_hiermoe_adaln"


def rmsnorm(x, g):
    xf = x.astype(jnp.float32)
    y = xf * lax.rsqrt(jnp.mean(xf * xf, axis=-1, keepdims=True) + NORM_EPS)
    return y.astype(x.dtype) * g


def modulate(xn, shift, scale):
    return xn * (1.0 + scale[:, None, :]) + shift[:, None, :]


def rope_partial(x, pos):
    d = x.shape[-1]
    rd = d // ROPE_FRACTION
    half = rd // 2
    inv = jnp.float32(ROPE_THETA) ** (-(jnp.arange(half, dtype=jnp.float32) * 2.0) / rd)
    ang = pos[:, None] * inv[None, :]
    cos = jnp.cos(ang)[None, :, None, :]
    sin = jnp.sin(ang)[None, :, None, :]
    xr = x[..., :rd].astype(jnp.float32)
    x1, x2 = xr[..., :half], xr[..., half:]
    rot = jnp.concatenate([x1 * cos - x2 * sin, x2 * cos + x1 * sin], axis=-1).astype(x.dtype)
    return jnp.concatenate([rot, x[..., rd:]], axis=-1)


def dsa_attention(q, k, v, qi, ki, wi):
    B, S = q.shape[0], q.shape[1]
    n_sel = min(TOPK_MAX, S // 4)
    nb = S // Q_BLOCK
    rep = N_HEADS // N_KV_HEADS

    def to_blocks(a):
        return jnp.moveaxis(a.reshape((B, nb, Q_BLOCK) + a.shape[2:]), 1, 0)

    t_blocks = jnp.arange(S, dtype=jnp.int32).reshape(nb, Q_BLOCK)
    s_pos = jnp.arange(S, dtype=jnp.int32)
    b_ix = jnp.arange(B)[:, None, None]
    idx_scale = (IDX_DIM ** -0.5) * (IDX_HEADS ** -0.5)
    attn_scale = HEAD_DIM ** -0.5
    kif = ki.astype(jnp.float32)

    def one_block(args):
        qb, qib, wib, tb = args
        logits = jnp.einsum('bthd,bsd->bths', qib.astype(jnp.float32), kif)
        score = jnp.einsum('bths,bth->bts', jax.nn.relu(logits), wib.astype(jnp.float32)) * idx_scale
        causal = s_pos[None, :] <= tb[:, None]
        score = jnp.where(causal[None], score, -jnp.inf)
        _, sel = lax.top_k(score, n_sel)
        valid = sel <= tb[None, :, None]
        kg = k[b_ix, sel].astype(jnp.float32)
        vg = v[b_ix, sel].astype(jnp.float32)
        qg = qb.reshape(B, Q_BLOCK, N_KV_HEADS, rep, HEAD_DIM).astype(jnp.float32)
        s = jnp.einsum('btgrd,btkgd->btgrk', qg, kg) * attn_scale
        s = jnp.where(valid[:, :, None, None, :], s, -jnp.inf)
        p = jax.nn.softmax(s, axis=-1)
        o = jnp.einsum('btgrk,btkgd->btgrd', p, vg).astype(qb.dtype)
        return o.reshape(B, Q_BLOCK, ATTN_WIDTH)

    out = lax.map(one_block, (to_blocks(q), to_blocks(qi), to_blocks(wi), t_blocks))
    return jnp.moveaxis(out, 0, 1).reshape(B, S, ATTN_WIDTH)


def multiscale_pool(p, w_pool, pool_scale):
    B, S = p.shape[0], p.shape[1]
    pg = p.reshape(B, S, POOL_GROUPS, POOL_GROUP_DIM).astype(jnp.float32)
    cs = jnp.cumsum(pg, axis=1)
    cs_pad = jnp.pad(cs, ((0, 0), (1, 0), (0, 0), (0, 0)))
    win = jnp.array(POOL_WINDOWS, dtype=jnp.int32)
    t = jnp.arange(S, dtype=jnp.int32)[:, None]
    lo = jnp.maximum(t + 1 - win[None, :], 0)
    lag = cs_pad[:, lo, jnp.arange(POOL_GROUPS)[None, :], :]
    count = jnp.minimum(t + 1, win[None, :]).astype(jnp.float32)
    mixed = (cs - lag) / count[None, :, :, None] - pg
    y = jnp.einsum('bsgc,gcd->bsgd', mixed.astype(p.dtype), w_pool) * pool_scale
    return y.reshape(B, S, POOL_WIDTH)


def hier_moe(h, w_grp, w_exp, w1, w3, w2):
    B, S, D = h.shape
    ht = h.reshape(-1, D)
    pg = jax.nn.softmax((ht @ w_grp).astype(jnp.float32), axis=-1)
    g_sel = jnp.argmax(pg, axis=-1)
    p_g = jnp.max(pg, axis=-1)
    el = (ht @ w_exp).astype(jnp.float32).reshape(-1, N_EXPERT_GROUPS, EXPERTS_PER_GROUP)
    el_sel = jnp.take_along_axis(el, g_sel[:, None, None], axis=1)[:, 0]
    pe = jax.nn.softmax(el_sel, axis=-1)
    top_p, top_i = lax.top_k(pe, EXPERT_TOPK)
    top_w = top_p / jnp.sum(top_p, axis=-1, keepdims=True) * p_g[:, None]
    eid = g_sel[:, None] * EXPERTS_PER_GROUP + top_i
    combine = jnp.einsum('tke,tk->te', jax.nn.one_hot(eid, N_EXPERTS, dtype=jnp.float32), top_w)
    y = jnp.zeros((ht.shape[0], D), jnp.float32)
    for e in range(N_EXPERTS):
        a = jax.nn.silu(ht @ w1[e]) * (ht @ w3[e])
        y = y + combine[:, e:e + 1] * (a @ w2[e]).astype(jnp.float32)
    return y.astype(h.dtype).reshape(B, S, D)


def setup_inputs(seed: int = 0) -> dict:
    key = jax.random.key(seed)
    ks = jax.random.split(key, 20)
    f32 = jnp.float32
    L, D = DEPTH, D_MODEL
    nrm = lambda k, shape, s: jax.random.normal(k, shape, f32) * s
    return {
        "x": nrm(ks[0], (BATCH, SEQ, D), 1.0),
        "c": nrm(ks[1], (BATCH, D), 1.0),
        "w_ada": nrm(ks[2], (L, D, 6 * D), 0.5 * D ** -0.5),
        "b_ada": nrm(ks[3], (L, 6 * D), 0.02),
        "norm1_g": 1.0 + nrm(ks[4], (L, D), 0.02),
        "w_in": nrm(ks[5], (L, D, PROJ_WIDTH), D ** -0.5),
        "q_norm_g": 1.0 + nrm(ks[6], (L, HEAD_DIM), 0.02),
        "k_norm_g": 1.0 + nrm(ks[7], (L, HEAD_DIM), 0.02),
        "w_pool": nrm(ks[8], (L, POOL_GROUPS, POOL_GROUP_DIM, POOL_GROUP_DIM), POOL_GROUP_DIM ** -0.5),
        "pool_scale": 1.0 + nrm(ks[9], (L, POOL_GROUPS, POOL_GROUP_DIM), 0.02),
        "w_out": nrm(ks[10], (L, MIX_WIDTH, D), MIX_WIDTH ** -0.5),
        "norm2_g": 1.0 + nrm(ks[11], (L, D), 0.02),
        "w_grp": nrm(ks[12], (L, D, N_EXPERT_GROUPS), D ** -0.5),
        "w_exp": nrm(ks[13], (L, D, N_EXPERTS), D ** -0.5),
        "w1": nrm(ks[14], (L, N_EXPERTS, D, D_EXPERT), D ** -0.5),
        "w3": nrm(ks[15], (L, N_EXPERTS, D, D_EXPERT), D ** -0.5),
        "w2": nrm(ks[16], (L, N_EXPERTS, D_EXPERT, D), D_EXPERT ** -0.5),
    }


def reference(x, c, w_ada, b_ada, norm1_g, w_in, q_norm_g, k_norm_g, w_pool, pool_scale,
              w_out, norm2_g, w_grp, w_exp, w1, w3, w2):
    B, S, D = x.shape
    pos = jnp.arange(S, dtype=jnp.float32)
    offsets = [int(o) for o in np.cumsum(PROJ_SPLITS)[:-1]]
    for l in range(DEPTH):
        ada = jax.nn.silu(c) @ w_ada[l] + b_ada[l]
        sh1, sc1, gt1, sh2, sc2, gt2 = jnp.split(ada, 6, axis=-1)
        h = modulate(rmsnorm(x, norm1_g[l]), sh1, sc1)
        proj = h @ w_in[l]
        q, k, v, qi, ki, wi, p = jnp.split(proj, offsets, axis=-1)
        q = rope_partial(rmsnorm(q.reshape(B, S, N_HEADS, HEAD_DIM), q_norm_g[l]), pos)
        k = rope_partial(rmsnorm(k.reshape(B, S, N_KV_HEADS, HEAD_DIM), k_norm_g[l]), pos)
        v = v.reshape(B, S, N_KV_HEADS, HEAD_DIM)
        qi = rope_partial(qi.reshape(B, S, IDX_HEADS, IDX_DIM), pos)
        ki = rope_partial(ki.reshape(B, S, 1, IDX_DIM), pos)[:, :, 0]
        attn_out = dsa_attention(q, k, v, qi, ki, wi)
        pool_out = multiscale_pool(p, w_pool[l], pool_scale[l])
        mix = jnp.concatenate([attn_out, pool_out], axis=-1) @ w_out[l]
        x = x + gt1[:, None, :] * mix
        h2 = modulate(rmsnorm(x, norm2_g[l]), sh2, sc2)
        x = x + gt2[:, None, :] * hier_moe(h2, w_grp[l], w_exp[l], w1[l], w3[l], w2[l])
    return x
```

```python
from contextlib import ExitStack
import numpy as np
import ml_dtypes
import concourse.bass as bass
import concourse.mybir as mybir
from concourse.bass_utils import run_bass_kernel_spmd

F32 = mybir.dt.float32
BF16 = mybir.dt.bfloat16
AF = mybir.ActivationFunctionType
ALU = mybir.AluOpType
AX = mybir.AxisListType

D = 2048
KC = 16
NH = 8
NG = 2
HD = 128
IH = 16
IDM = 64
PW = 1024
PROJ = 3664
NEXP = 32
DEXP = 512
EPS = 1e-6
NEG = -1.0e30
TOPK = 256
N_BISECT = 18
C_Q, C_K, C_V, C_QI, C_KI, C_WI, C_P = 0, 1024, 1280, 1536, 2560, 2624, 2640


class Tok:
    __slots__ = ("name", "w", "r", "dsem")

    def __init__(self, name):
        self.name = name
        self.w = None
        self.r = {}
        self.dsem = None


class Sched:
    ENG = ("pe", "act", "dve", "pool", "sp")

    def __init__(self, nc, stack):
        self.nc = nc
        self.stack = stack
        self.sems = {}
        self.count = {}
        self.waited = {e: {} for e in self.ENG}
        self.prog = {e: [] for e in self.ENG}
        for e in self.ENG:
            self._mksem(e)
        self.free_dsems = []
        self.n_ops = 0

    def _mksem(self, key):
        s = self.stack.enter_context(self.nc.semaphore("s_" + str(key)))
        self.sems[key] = s
        self.count[key] = 0
        return s

    def tok(self, name):
        return Tok(name)

    def toks(self, name, n):
        return [Tok("%s%d" % (name, i)) for i in range(n)]

    def _need(self, eng, ev, waits, raw=False):
        if ev is None:
            return
        k, v = ev
        if k == eng and eng == "pe":
            return
        if self.waited[eng].get(k, 0) >= v:
            return
        if waits.get(k, 0) < v:
            waits[k] = v

    def _deps(self, eng, reads, writes):
        waits = {}
        for t in reads:
            self._need(eng, t.w, waits, raw=True)
        for t in writes:
            self._need(eng, t.w, waits)
            for k, v in t.r.items():
                self._need(eng, (k, v), waits)
        for k, v in waits.items():
            self.waited[eng][k] = v
        return waits

    def op(self, eng, fn, reads=(), writes=()):
        waits = self._deps(eng, reads, writes)
        self.count[eng] += 1
        v = self.count[eng]
        self.prog[eng].append((fn, waits, (eng, 1)))
        for t in reads:
            t.r[eng] = v
        for t in writes:
            t.w = (eng, v)
            t.r = {}
        self.n_ops += 1
        return v

    def dma(self, eng, fn, reads=(), writes=(), owner=None):
        assert owner is not None
        if owner.dsem is None:
            owner.dsem = "d%d" % len(self.sems)
            self._mksem(owner.dsem)
        k = owner.dsem
        waits = self._deps(eng, reads, writes)
        if self.count[k] > 0 and self.waited[eng].get(k, 0) < self.count[k]:
            waits[k] = self.count[k]
            self.waited[eng][k] = self.count[k]
        self.count[k] += 16
        v = self.count[k]
        self.prog[eng].append((fn, waits, (k, 16)))
        for t in reads:
            t.r[k] = v
        for t in writes:
            t.w = (k, v)
            t.r = {}
        self.n_ops += 1
        return (k, v)

    def barrier(self, engines=None):
        engines = engines or self.ENG
        for e in engines:
            waits = {}
            for k, c in self.count.items():
                if k == e or c == 0:
                    continue
                if self.waited[e].get(k, 0) < c:
                    waits[k] = c
                    self.waited[e][k] = c
            if waits:
                self.prog[e].append((None, waits, None))

    def emit(self, block):
        sems = self.sems

        def run(e_obj, prog):
            for fn, waits, inc in prog:
                for k, v in waits.items():
                    e_obj.wait_ge(sems[k], v)
                if fn is None:
                    continue
                ins = fn(e_obj)
                ins.then_inc(sems[inc[0]], inc[1])

        @block.tensor
        def _(e):
            run(e, self.prog["pe"])

        @block.scalar
        def _(e):
            run(e, self.prog["act"])

        @block.vector
        def _(e):
            run(e, self.prog["dve"])

        @block.gpsimd
        def _(e):
            run(e, self.prog["pool"])

        @block.sync
        def _(e):
            run(e, self.prog["sp"])


class Prog:
    def __init__(self, SEQ, debug=False, stop_after=None):
        self.SEQ = SEQ
        self.NL = SEQ // 512
        self.NOWN = self.NL // 2
        self.NBO = self.NOWN * 4
        self.NBA = SEQ // 128
        self.debug = debug
        self.stop_after = stop_after
        self.nc = bass.Bass("TRN2", target_bir_lowering=False)

    def din(self, name, shape, dt=F32):
        return self.nc.dram_tensor(name, list(shape), dt, kind="ExternalInput").ap()

    def dscr(self, name, shape, dt):
        kind = "ExternalOutput" if self.debug else "Internal"
        return self.nc.dram_tensor(name, list(shape), dt, kind=kind).ap()

    def sb(self, st, name, shape, dt):
        self._uid = getattr(self, "_uid", 0) + 1
        return st.enter_context(self.nc.sbuf_tensor("sb%d_%s" % (self._uid, name), list(shape), dt))

    def ps(self, st, name, shape, dt):
        self._uid = getattr(self, "_uid", 0) + 1
        return st.enter_context(self.nc.psum_tensor("ps%d_%s" % (self._uid, name), list(shape), dt))

    def mm(self, out, lhsT, rhs, start, stop, reads, writes):
        self.S.op("pe", lambda e: e.matmul(out, lhsT=lhsT, rhs=rhs, start=start, stop=stop), reads=reads, writes=writes)

    def tr(self, out, in_, ident, reads, writes):
        self.S.op("pe", lambda e: e.transpose(out, in_, ident), reads=reads, writes=writes)

    def act(self, out, in_, func, reads, writes, bias=None, scale=1.0, accum_out=None, eng="act"):
        kw = {}
        if bias is not None:
            kw["bias"] = bias
        if accum_out is not None:
            kw["accum_out"] = accum_out
        self.S.op(eng, lambda e: e.activation(out=out, in_=in_, func=func, scale=scale, **kw), reads=reads, writes=writes)

    def ts(self, eng, out, in0, s1, s2, op0, op1, reads, writes, accum_out=None):
        kw = {}
        if accum_out is not None:
            kw["accum_out"] = accum_out
        if op1 is None:
            self.S.op(eng, lambda e: e.tensor_scalar(out=out, in0=in0, scalar1=s1, scalar2=None, op0=op0, **kw), reads=reads, writes=writes)
        else:
            self.S.op(eng, lambda e: e.tensor_scalar(out=out, in0=in0, scalar1=s1, scalar2=s2, op0=op0, op1=op1, **kw), reads=reads, writes=writes)

    def tt(self, eng, out, in0, in1, op, reads, writes):
        self.S.op(eng, lambda e: e.tensor_tensor(out=out, in0=in0, in1=in1, op=op), reads=reads, writes=writes)

    def stt(self, eng, out, in0, scalar, in1, op0, op1, reads, writes):
        self.S.op(eng, lambda e: e.scalar_tensor_tensor(out=out, in0=in0, scalar=scalar, in1=in1, op0=op0, op1=op1), reads=reads, writes=writes)

    def cp(self, eng, out, in_, reads, writes):
        if eng == "act":
            self.S.op(eng, lambda e: e.activation(out=out, in_=in_, func=AF.Copy), reads=reads, writes=writes)
        else:
            self.S.op(eng, lambda e: e.tensor_copy(out=out, in_=in_), reads=reads, writes=writes)

    def ld(self, out, in_, writes, owner, reads=(), eng="sp", **kw):
        return self.S.dma(eng, lambda e: e.dma_start(out=out, in_=in_, **kw), reads=reads, writes=writes, owner=owner)

    def stg(self, out, in_, reads, owner, writes=(), eng="sp"):
        return self.S.dma(eng, lambda e: e.dma_start(out=out, in_=in_), reads=reads, writes=writes, owner=owner)

    def declare(self):
        SEQ, NBO = self.SEQ, self.NBO
        i = {}
        i["x_loc"] = self.din("x_loc", [SEQ, D])
        i["x_halo"] = self.din("x_halo", [128, D])
        i["cvec"] = self.din("cvec", [128, KC])
        i["rope"] = self.din("rope", [SEQ, 48])
        i["maskadd"] = self.din("maskadd", [128, 4, 1024], BF16)
        i["poolA"] = self.din("poolA", [128, 12 + 4 * self.NOWN, 128], BF16)
        i["ident_f"] = self.din("ident_f", [128, 128])
        i["ident_b"] = self.din("ident_b", [128, 128], BF16)
        i["rrep"] = self.din("rrep", [16, 128], BF16)
        i["maski"] = self.din("maski", [128, 16, 128], BF16)
        i["w_ada"] = self.din("w_ada", [D, 6 * D])
        i["b_ada"] = self.din("b_ada", [1, 6 * D])
        i["g1col"] = self.din("g1col", [128, KC])
        i["g2col"] = self.din("g2col", [128, KC])
        i["w_in"] = self.din("w_in", [D, PROJ])
        i["qg"] = self.din("qg", [1, 128])
        i["kg"] = self.din("kg", [1, 128])
        i["w_pool"] = self.din("w_pool", [4, 256, 256])
        i["pscol"] = self.din("pscol", [128, 8])
        i["w_out"] = self.din("w_out", [D, D])
        i["w_rt"] = self.din("w_rt", [D, 36])
        i["w1"] = self.din("w1", [NEXP, D, DEXP])
        i["w3"] = self.din("w3", [NEXP, D, DEXP])
        i["w2"] = self.din("w2", [NEXP, DEXP, D])
        self.i = i
        self.out = self.nc.dram_tensor("out", [self.NOWN * 512, D], F32, kind="ExternalOutput").ap()
        s = {}
        s["kT"] = self.dscr("kT_s", [128, NG, SEQ], BF16)
        s["v"] = self.dscr("v_s", [SEQ, 256], BF16)
        s["kiT"] = self.dscr("kiT_s", [128, SEQ], BF16)
        s["qT"] = self.dscr("qT_s", [NBO, 128, NH, 128], BF16)
        s["qiT"] = self.dscr("qiT_s", [NBO, 128, 16, 128], BF16)
        s["wbd"] = self.dscr("wbd_s", [NBO, 128, 16, 128], BF16)
        s["mixT"] = self.dscr("mixT_s", [NBO, 128, KC, 128], BF16)
        s["x1"] = self.dscr("x1_s", [NBO * 128, D], F32)
        s["h2T"] = self.dscr("h2T_s", [NBO, 128, KC, 128], BF16)
        s["gt"] = self.dscr("gt_s", [2, D], F32)
        self.s = s
        self.st_tok = {k: Tok("scr_" + k) for k in s}

    def build(self):
        nc = self.nc
        self.declare()
        with ExitStack() as top:
            self.S = S = Sched(nc, top)
            self.cols = self.sb(top, "cols", [128, 96], F32)
            self.t_cols = S.tok("cols")
            self.G1 = self.sb(top, "G1", [128, KC], F32)
            self.G2 = self.sb(top, "G2", [128, KC], F32)
            self.t_G = S.tok("G")
            self.ident_f = self.sb(top, "ident_f", [128, 128], F32)
            self.ident_b = self.sb(top, "ident_b", [128, 128], BF16)
            self.ones_f = self.sb(top, "ones_f", [128, 128], F32)
            self.ones_b = self.sb(top, "ones_b", [128, 128], BF16)
            self.t_const = S.tok("const")
            tcf, tcb = S.tok("cf"), S.tok("cb")
            self.ld(self.ident_f[:], self.i["ident_f"], [tcf], tcf)
            self.ld(self.ident_b[:], self.i["ident_b"], [tcb], tcb)
            S.op("dve", lambda e: e.memset(self.ones_f[:], 1.0), reads=[tcf, tcb], writes=[self.t_const])
            S.op("dve", lambda e: e.memset(self.ones_b[:], 1.0), writes=[self.t_const])
            self.comb = self.sb(top, "comb", [128, self.NBO, NEXP], F32)
            self.t_comb = S.tok("comb")

            self.phase_a()
            S.barrier()
            if self.stop_after != "a":
                self.phase_b()
                S.barrier()
            if self.stop_after not in ("a", "b"):
                self.phase_c()
                S.barrier()
            if self.stop_after not in ("a", "b", "c"):
                self.phase_d()
                S.barrier()
            if self.stop_after not in ("a", "b", "c", "d"):
                self.phase_e()
            if self.stop_after is not None:
                tdb = S.tok("dbg")
                self.stg(self.out[0:128, 0:96], self.cols[:], [self.t_cols], tdb)
            S.barrier()
            with nc.Block() as block:
                S.emit(block)
        return nc

    def phase_a(self):
        S = self.S
        with ExitStack() as st:
            cv = self.sb(st, "cv", [128, KC], F32)
            sc = self.sb(st, "sc", [128, KC], F32)
            crep = self.sb(st, "crep", [128, KC, 128], F32)
            t_cv, t_sc, t_crep = S.tok("cv"), S.tok("sc"), S.tok("crep")
            wa = [self.sb(st, "wa%d" % k, [128, KC, 512], F32) for k in range(2)]
            t_wa = S.toks("wa", 2)
            ba = self.sb(st, "ba", [1, 6 * D], F32)
            t_ba = S.tok("ba")
            ada = self.sb(st, "ada", [128, 6 * D], F32)
            t_ada = S.tok("ada")
            g1c = self.sb(st, "g1c", [128, KC], F32)
            g2c = self.sb(st, "g2c", [128, KC], F32)
            t_g1c, t_g2c = S.tok("g1c"), S.tok("g2c")
            pa = [self.ps(st, "pa%d" % k, [128, 512], F32) for k in range(2)]
            t_pa = S.toks("pa", 2)

            self.ld(cv[:], self.i["cvec"], [t_cv], t_cv)
            self.ld(ba[:], self.i["b_ada"], [t_ba], t_ba)
            self.ld(g1c[:], self.i["g1col"], [t_g1c], t_g1c)
            self.ld(g2c[:], self.i["g2col"], [t_g2c], t_g2c)
            self.act(sc[:], cv[:], AF.Silu, [t_cv], [t_sc])
            self.cp("dve", crep[:], sc[:].unsqueeze(2).to_broadcast([128, KC, 128]), [t_sc], [t_crep])
            wv = self.i["w_ada"].rearrange("(k p) n -> p k n", p=128)
            for nb in range(24):
                b = nb % 2
                self.ld(wa[b][:], wv[:, :, nb * 512:(nb + 1) * 512], [t_wa[b]], t_wa[b])
                for kc in range(KC):
                    self.mm(pa[b][:], crep[:, kc, :], wa[b][:, kc, :], kc == 0, False, [t_crep, t_wa[b]], [t_pa[b]])
                self.mm(pa[b][:], self.ones_f[0:1, :], ba[0:1, nb * 512:(nb + 1) * 512], False, True,
                        [self.t_const, t_ba], [t_pa[b]])
                self.cp("act" if nb % 2 else "dve", ada[:, nb * 512:(nb + 1) * 512], pa[b][:], [t_pa[b]], [t_ada])
            for hf in range(2):
                tmpv = wa[hf][:].rearrange("p k n -> p (k n)")[:, 0:48 * 128].rearrange("p (j i) -> p j i", i=128)
                self.tt("dve", tmpv, ada[:, hf * 6144:(hf + 1) * 6144].rearrange("p (j i) -> p j i", i=128),
                        self.ident_f[:].unsqueeze(1).to_broadcast([128, 48, 128]), ALU.mult,
                        [t_ada, self.t_const], [t_wa[hf]])
                S.op("dve", lambda e, tmpv=tmpv, hf=hf: e.tensor_reduce(out=self.cols[:, hf * 48:(hf + 1) * 48], in_=tmpv, axis=AX.X, op=ALU.add),
                     reads=[t_wa[hf]], writes=[self.t_cols])
            self.stt("dve", self.G1[:], self.cols[:, 16:32], 1.0, g1c[:], ALU.add, ALU.mult, [self.t_cols, t_g1c], [self.t_G])
            self.stt("dve", self.G2[:], self.cols[:, 64:80], 1.0, g2c[:], ALU.add, ALU.mult, [self.t_cols, t_g2c], [self.t_G])
            self.stg(self.s["gt"][0:1, :], ada[0:1, 2 * D:3 * D], [t_ada], t_ada, writes=[self.st_tok["gt"]])
            self.stg(self.s["gt"][1:2, :], ada[0:1, 5 * D:6 * D], [t_ada], t_ada, writes=[self.st_tok["gt"]])

    def norm_block(self, xt, t_xt, Gc, SHc, hT, t_hT, R, hTf=None, t_hTf=None):
        self.act(R["junk"][:], xt[:], AF.Square, [t_xt], [R["t_junk"], R["t_ss"]], accum_out=R["ss"][:, 0:1])
        self.act(R["ss"][:, 1:2], R["ss"][:, 0:1], AF.Sqrt, [R["t_ss"], R["t_eps"]], [R["t_ss"]], bias=R["epsc"][:, 0:1], scale=1.0 / D)
        self.S.op("dve", lambda e: e.reciprocal(out=R["ss"][:, 2:3], in_=R["ss"][:, 1:2]), reads=[R["t_ss"]], writes=[R["t_ss"]])
        self.act(R["xs"][:], xt[:], AF.Copy, [t_xt, R["t_ss"]], [R["t_xs"]], scale=R["ss"][:, 2:3])
        for kc in range(KC):
            self.tr(R["xtp"][:, kc * 128:(kc + 1) * 128], R["xs"][:, kc * 128:(kc + 1) * 128], self.ident_f[:],
                    [R["t_xs"], self.t_const], [R["t_xtp"][kc // 4]])
        for kc in range(KC):
            if hTf is None:
                self.ts("dve", hT[:, kc, :], R["xtp"][:, kc * 128:(kc + 1) * 128], Gc[:, kc:kc + 1], SHc[:, kc:kc + 1],
                        ALU.mult, ALU.add, [R["t_xtp"][kc // 4], self.t_G, self.t_cols], [t_hT])
            else:
                self.ts("dve", hTf[:, kc, :], R["xtp"][:, kc * 128:(kc + 1) * 128], Gc[:, kc:kc + 1], SHc[:, kc:kc + 1],
                        ALU.mult, ALU.add, [R["t_xtp"][kc // 4], self.t_G, self.t_cols], [t_hTf])
        if hTf is not None:
            self.cp("act", hT[:].rearrange("p k t -> p (k t)"), hTf[:].rearrange("p k t -> p (k t)"), [t_hTf], [t_hT])

    def rope_ops(self, src, dst, H, half, cos, sin, tmp, reads, t_dst, t_tmp, eng="dve"):
        x1 = src[:, :, 0:half]
        x2 = src[:, :, half:2 * half]
        cb = cos.unsqueeze(1).to_broadcast([128, H, half])
        sn = sin.unsqueeze(1).to_broadcast([128, H, half])
        t = [tmp[:, k, 0:H, 0:half] for k in range(4)]
        self.tt(eng, t[0], x1, cb, ALU.mult, reads, [t_tmp])
        self.tt(eng, t[1], x2, sn, ALU.mult, reads, [t_tmp])
        self.tt(eng, t[2], x2, cb, ALU.mult, reads, [t_tmp])
        self.tt(eng, t[3], x1, sn, ALU.mult, reads, [t_tmp])
        self.tt(eng, dst[:, :, 0:half], t[0], t[1], ALU.subtract, [t_tmp], [t_dst])
        self.tt(eng, dst[:, :, half:2 * half], t[2], t[3], ALU.add, [t_tmp], [t_dst])

    def phase_b(self):
        S, i, s = self.S, self.i, self.s
        NOWN = self.NOWN
        with ExitStack() as st:
            w_own = self.sb(st, "w_own", [128, KC, 3088], BF16)
            w_kv = self.sb(st, "w_kv", [128, KC, 576], BF16)
            t_wown, t_wkv = S.tok("w_own"), S.tok("w_kv")
            wv = i["w_in"].rearrange("(k p) n -> p k n", p=128)
            t_wparts = S.toks("wpart", 6)
            self.ld(w_kv[:, :, 0:512], wv[:, :, C_K:C_K + 512], [t_wkv], t_wkv, eng="pool")
            self.ld(w_kv[:, :, 512:576], wv[:, :, C_KI:C_KI + 64], [t_wkv], t_wkv, eng="pool")
            self.ld(w_own[:, :, 0:1024], wv[:, :, C_Q:C_Q + 1024], [t_wown], t_wown, eng="pool")
            self.ld(w_own[:, :, 1024:2048], wv[:, :, C_QI:C_QI + 1024], [t_wown], t_wown, eng="pool")
            self.ld(w_own[:, :, 2048:2064], wv[:, :, C_WI:C_WI + 16], [t_wown], t_wown, eng="pool")
            self.ld(w_own[:, :, 2064:3088], wv[:, :, C_P:C_P + 1024], [t_wown], t_wown, eng="pool")
            wpool = self.sb(st, "wpool", [128, 4, 2, 256], BF16)
            t_wpool = S.tok("wpool")
            self.ld(wpool[:], i["w_pool"].rearrange("g (c p) d -> p g c d", p=128), [t_wpool], t_wpool, eng="pool")
            pscol = self.sb(st, "pscol", [128, 8], F32)
            t_ps = S.tok("pscol")
            self.ld(pscol[:], i["pscol"], [t_ps], t_ps)
            poolA = self.sb(st, "poolA", [128, 12 + 4 * NOWN, 128], BF16)
            t_pA = S.tok("poolA")
            self.ld(poolA[:], i["poolA"], [t_pA], t_pA)
            rrep = self.sb(st, "rrep", [16, 128], BF16)
            maski = self.sb(st, "maski", [128, 16, 128], BF16)
            t_rr, t_mi = S.tok("rrep"), S.tok("maski")
            self.ld(rrep[:], i["rrep"], [t_rr], t_rr)
            self.ld(maski[:], i["maski"], [t_mi], t_mi)
            qg = self.sb(st, "qg", [128, 128], F32)
            kg = self.sb(st, "kg", [128, 128], F32)
            t_qg, t_kg = S.tok("qg"), S.tok("kg")
            self.ld(qg[:], i["qg"].partition_broadcast(128), [t_qg], t_qg)
            self.ld(kg[:], i["kg"].partition_broadcast(128), [t_kg], t_kg)
            self.ts("dve", qg[:], qg[:], float(HD ** -0.5), None, ALU.mult, None, [t_qg], [t_qg])
            epsc = self.sb(st, "epsc", [128, 1], F32)
            t_eps = S.tok("epsc")
            S.op("dve", lambda e: e.memset(epsc[:], EPS), writes=[t_eps])

            xt = [self.sb(st, "xt%d" % k, [128, D], F32) for k in range(2)]
            t_xt = S.toks("xt", 2)
            R = {
                "ss": self.sb(st, "ss", [128, 4], F32), "t_ss": S.tok("ss"),
                "xs": self.sb(st, "xs", [128, D], F32), "t_xs": S.tok("xs"),
                "xtp": self.ps(st, "xtp", [128, D], F32), "t_xtp": S.toks("xtp", 4),
                "epsc": epsc, "t_eps": t_eps,
            }
            R["junk"], R["t_junk"] = R["xs"], R["t_xs"]
            hT = [self.sb(st, "hT%d" % k, [128, KC, 128], BF16) for k in range(2)]
            t_hT = S.toks("hT", 2)
            rp = [self.sb(st, "rp%d" % k, [128, 48], F32) for k in range(2)]
            t_rp = S.toks("rp", 2)
            pj = [self.ps(st, "pj%d" % k, [128, 512], F32) for k in range(2)]
            t_pj = S.toks("pj", 2)
            tq = self.ps(st, "tq", [128, 1024], BF16)
            t_tq = S.tok("tq")
            pm = self.ps(st, "pm", [128, 512], F32)
            t_pm = S.tok("pm")
            hss = self.sb(st, "hss", [128, 16], F32)
            t_hss = S.tok("hss")
            kn = self.sb(st, "kn", [128, NG, 128], F32)
            knb = self.sb(st, "knb", [128, NG, 128], BF16)
            t_kn, t_knb = S.tok("kn"), S.tok("knb")
            qn = self.sb(st, "qn", [128, NH, 128], F32)
            qnb = self.sb(st, "qnb", [128, NH, 128], BF16)
            t_qn, t_qnb = S.tok("qn"), S.tok("qnb")
            qif = qn[:].rearrange("p h d -> p (h d)").rearrange("p (h d) -> p h d", d=IDM)
            qib = qnb[:].rearrange("p h d -> p (h d)").rearrange("p (h d) -> p h d", d=IDM)
            t_qif, t_qib = t_qn, t_qnb
            kif = self.sb(st, "kif", [128, 1, IDM], F32)
            kib = self.sb(st, "kib", [128, 2, IDM], BF16)
            t_kif, t_kib = S.tok("kif"), S.tok("kib")
            rtmp = self.sb(st, "rtmp", [128, 4, 16, 16], F32)
            t_rtmp = S.tok("rtmp")
            vb = self.sb(st, "vb", [128, 256], BF16)
            t_vb = S.tok("vb")
            kst = self.sb(st, "kst", [128, NG, 128], BF16)
            t_kst = S.tok("kst")
            kist = self.sb(st, "kist", [128, 128], BF16)
            t_kist = S.tok("kist")
            qst = self.sb(st, "qst", [128, NH, 128], BF16)
            t_qst = S.tok("qst")
            qist = self.sb(st, "qist", [128, 16, 128], BF16)
            t_qist = S.tok("qist")
            S.op("pool", lambda e: e.memset(qist[:], 0.0), writes=[t_qist])
            wib = self.sb(st, "wib", [128, 16], BF16)
            wT = self.sb(st, "wT", [16, 128], BF16)
            t_wib, t_wT = S.tok("wib"), S.tok("wT")
            wbd = self.sb(st, "wbd", [128, 16, 128], BF16)
            t_wbd = S.tok("wbd")
            pcur = [self.sb(st, "pcur%d" % k, [128, PW], BF16) for k in range(2)]
            t_pcur = S.toks("pcur", 2)
            phalo = self.sb(st, "phalo", [128, PW], BF16)
            t_phalo = S.tok("phalo")
            mixsb = self.sb(st, "mixsb", [128, 8, 128], BF16)
            t_mixsb = S.tok("mixsb")
            mixst = self.sb(st, "mixst", [128, 8, 128], BF16)
            t_mixst = S.tok("mixst")
            SH1 = self.cols[:, 0:16]

            def proj(dst_ps, t_dst, h, t_h, w, t_w, c0, n):
                for kc in range(KC):
                    self.mm(dst_ps[:, 0:n], h[:, kc, :], w[:, kc, c0:c0 + n], kc == 0, kc == KC - 1, [t_h, t_w], [t_dst])

            def head_norm(ps_ap, H, gain, t_gain, dst, t_dst, t_ps, col0):
                for hh in range(H):
                    self.act(R["junk"][:, hh * 128:(hh + 1) * 128], ps_ap[:, hh * 128:(hh + 1) * 128], AF.Square,
                             [t_ps], [R["t_junk"], t_hss], accum_out=hss[:, hh:hh + 1])
                self.act(hss[:, 4:4 + H], hss[:, 0:H], AF.Sqrt, [t_hss, t_eps], [t_hss], bias=epsc[:, 0:1], scale=1.0 / HD)
                S.op("dve", lambda e: e.reciprocal(out=hss[:, 8:8 + H], in_=hss[:, 4:4 + H]), reads=[t_hss], writes=[t_hss])
                for hh in range(H):
                    self.stt("dve", dst[:, col0 + hh, :], ps_ap[:, hh * 128:(hh + 1) * 128], hss[:, 8 + hh:9 + hh], gain[:],
                             ALU.mult, ALU.mult, [t_ps, t_hss, t_gain], [t_dst])

            def kv_part(blk, b, rpb, t_rpb):
                tok0 = blk * 128
                A, B = pj[0], pj[1]
                proj(A, t_pj[0], hT[b], t_hT[b], w_kv, t_wkv, 0, 512)
                proj(B, t_pj[1], hT[b], t_hT[b], w_kv, t_wkv, 512, 64)
                head_norm(A[:, 0:256], NG, kg, t_kg, kn, t_kn, t_pj[0], 0)
                self.cp("act", vb[:], A[:, 256:512], [t_pj[0]], [t_vb])
                self.cp("act", kif[:, 0, :], B[:, 0:64], [t_pj[1]], [t_kif])
                self.cp("act", knb[:], kn[:], [t_kn], [t_knb])
                self.rope_ops(kn, knb, NG, 16, rpb[:, 0:16], rpb[:, 16:32], rtmp, [t_kn, t_rpb], t_knb, t_rtmp)
                self.cp("act", kib[:, 0, :], kif[:, 0, :], [t_kif], [t_kib])
                self.rope_ops(kif, kib[:, 0:1, :], 1, 8, rpb[:, 32:40], rpb[:, 40:48], rtmp, [t_kif, t_rpb], t_kib, t_rtmp)
                self.cp("act", kib[:, 1, :], kib[:, 0, :], [t_kib], [t_kib])
                for g in range(NG):
                    self.tr(tq[:, g * 128:(g + 1) * 128], knb[:, g, :], self.ident_b[:], [t_knb, self.t_const], [t_tq])
                self.tr(tq[:, 256:384], kib[:].rearrange("p a d -> p (a d)"), self.ident_b[:], [t_kib, self.t_const], [t_tq])
                self.cp("dve", kst[:].rearrange("p g t -> p (g t)"), tq[:, 0:256], [t_tq], [t_kst])
                self.cp("dve", kist[:], tq[:, 256:384], [t_tq], [t_kist])
                self.stg(s["kT"][:, :, tok0:tok0 + 128], kst[:], [t_kst], t_kst, writes=[self.st_tok["kT"]])
                self.stg(s["kiT"][:, tok0:tok0 + 128], kist[:], [t_kist], t_kist, writes=[self.st_tok["kiT"]])
                self.stg(s["v"][tok0:tok0 + 128, :], vb[:], [t_vb], t_vb, writes=[self.st_tok["v"]])

            def pool_proj(b, dst, t_dst):
                for half in range(2):
                    P_ = pj[half]
                    proj(P_, t_pj[half], hT[b], t_hT[b], w_own, t_wown, 2064 + half * 512, 512)
                    self.cp("act", dst[:, half * 512:(half + 1) * 512], P_[:], [t_pj[half]], [t_dst])

            def own_part(ob, b, rpb, t_rpb, pc, t_pc, pprev, t_pprev, a_cur, a_prev):
                for half in range(2):
                    P_ = pj[half]
                    proj(P_, t_pj[half], hT[b], t_hT[b], w_own, t_wown, half * 512, 512)
                    head_norm(P_[:], 4, qg, t_qg, qn, t_qn, t_pj[half], half * 4)
                self.cp("act", qnb[:], qn[:], [t_qn], [t_qnb])
                self.rope_ops(qn, qnb, NH, 16, rpb[:, 0:16], rpb[:, 16:32], rtmp, [t_qn, t_rpb], t_qnb, t_rtmp)
                for hh in range(NH):
                    self.tr(tq[:, hh * 128:(hh + 1) * 128], qnb[:, hh, :], self.ident_b[:], [t_qnb, self.t_const], [t_tq])
                self.cp("dve", qst[:].rearrange("p h t -> p (h t)"), tq[:], [t_tq], [t_qst])
                self.stg(s["qT"][ob], qst[:], [t_qst], t_qst, writes=[self.st_tok["qT"]])
                for half in range(2):
                    P_ = pj[half]
                    proj(P_, t_pj[half], hT[b], t_hT[b], w_own, t_wown, 1024 + half * 512, 512)
                    self.cp("act", qif[:, half * 8:(half + 1) * 8, :].rearrange("p h d -> p (h d)"), P_[:], [t_pj[half]], [t_qif])
                self.cp("act", qib[:], qif[:], [t_qif], [t_qib])
                self.rope_ops(qif, qib, IH, 8, rpb[:, 32:40], rpb[:, 40:48], rtmp, [t_qif, t_rpb], t_qib, t_rtmp)
                for c in range(8):
                    self.tr(tq[:, c * 128:(c + 1) * 128], qib[:, 2 * c:2 * c + 2, :].rearrange("p h d -> p (h d)"), self.ident_b[:],
                            [t_qib, self.t_const], [t_tq])
                for e_ in range(2):
                    self.cp("dve" if e_ == 0 else "act",
                            qist[e_ * 64:(e_ + 1) * 64, :, e_ * 64:(e_ + 1) * 64].rearrange("p g (c t) -> p c g t", c=8),
                            tq[e_ * 64:(e_ + 1) * 64, :].rearrange("p (c g t) -> p c g t", c=8, g=16), [t_tq], [t_qist])
                self.stg(s["qiT"][ob], qist[:], [t_qist], t_qist, writes=[self.st_tok["qiT"]])
                P_ = pj[0]
                proj(P_, t_pj[0], hT[b], t_hT[b], w_own, t_wown, 2048, 16)
                self.cp("act", wib[:], P_[:, 0:16], [t_pj[0]], [t_wib])
                self.tr(tq[0:16, 0:128], wib[:], self.ident_b[:], [t_wib, self.t_const], [t_tq])
                self.cp("dve", wT[:], tq[0:16, 0:128], [t_tq], [t_wT])
                P_ = pj[1]
                self.mm(P_[:, 0:128], rrep[:], wT[:], True, True, [t_rr, t_wT], [t_pj[1]])
                self.tt("dve", wbd[:], P_[:, 0:128].unsqueeze(1).to_broadcast([128, 16, 128]), maski[:], ALU.mult,
                        [t_pj[1], t_mi], [t_wbd])
                self.stg(s["wbd"][ob], wbd[:], [t_wbd], t_wbd, writes=[self.st_tok["wbd"]])
                pool_proj(b, pc, t_pc)
                for gp in range(2):
                    for gg in range(2):
                        g = gp * 2 + gg
                        for cc in range(2):
                            o_ = pm[:, (gg * 2 + cc) * 128:(gg * 2 + cc + 1) * 128]
                            ch = slice(g * 256 + cc * 128, g * 256 + (cc + 1) * 128)
                            self.mm(o_, pc[:, ch], poolA[:, a_cur + g, :], True, False, [t_pc, t_pA], [t_pm])
                            self.mm(o_, pprev[:, ch], poolA[:, a_prev + g, :], False, True, [t_pprev, t_pA], [t_pm])
                    self.cp("act", mixsb[:, gp * 4:(gp + 1) * 4, :].rearrange("p a t -> p (a t)"), pm[:], [t_pm], [t_mixsb])
                for g in range(4):
                    P_ = pj[g % 2]
                    for dc in range(2):
                        for cc in range(2):
                            self.mm(P_[:, dc * 128:(dc + 1) * 128], wpool[:, g, cc, dc * 128:(dc + 1) * 128], mixsb[:, g * 2 + cc, :],
                                    cc == 0, cc == 1, [t_wpool, t_mixsb], [t_pj[g % 2]])
                    for dc in range(2):
                        self.ts("dve", mixst[:, g * 2 + dc, :], P_[:, dc * 128:(dc + 1) * 128], pscol[:, g * 2 + dc:g * 2 + dc + 1], None,
                                ALU.mult, None, [t_pj[g % 2], t_ps], [t_mixst])
                self.stg(s["mixT"][ob, :, 8:16, :], mixst[:], [t_mixst], t_mixst, writes=[self.st_tok["mixT"]])

            self.ld(xt[0][:], i["x_halo"], [t_xt[0]], t_xt[0])
            self.norm_block(xt[0], t_xt[0], self.G1, SH1, hT[0], t_hT[0], R)
            pool_proj(0, phalo, t_phalo)

            def norm_stage(blk):
                b = blk % 2
                self.ld(xt[b][:], i["x_loc"][blk * 128:(blk + 1) * 128, :], [t_xt[b]], t_xt[b])
                self.ld(rp[b][:], i["rope"][blk * 128:(blk + 1) * 128, :], [t_rp[b]], t_rp[b])
                self.norm_block(xt[b], t_xt[b], self.G1, SH1, hT[b], t_hT[b], R)

            def proj_stage(blk):
                b = blk % 2
                chunk = blk // 4
                own = (chunk % 2 == 0)
                kv_part(blk, b, rp[b], t_rp[b])
                if own:
                    j = chunk // 2
                    bi = blk % 4
                    ob = j * 4 + bi
                    pb = ob % 2
                    if bi == 0:
                        a_cur = 8 if j == 0 else 0
                        own_part(ob, b, rp[b], t_rp[b], pcur[pb], t_pcur[pb], phalo, t_phalo, a_cur, 12 + 4 * j)
                    else:
                        own_part(ob, b, rp[b], t_rp[b], pcur[pb], t_pcur[pb], pcur[1 - pb], t_pcur[1 - pb], 0, 4)

            norm_stage(0)
            for blk in range(self.NBA):
                if blk + 1 < self.NBA:
                    norm_stage(blk + 1)
                proj_stage(blk)

    def phase_c(self):
        S, i, s = self.S, self.i, self.s
        SEQ = self.SEQ
        NKB = SEQ // 128
        NB = N_BISECT
        with ExitStack() as st:
            kiT = self.sb(st, "kiT", [128, SEQ], BF16)
            t_kiT = S.tok("kiT")
            self.ld(kiT[:], s["kiT"], [t_kiT], t_kiT, reads=[self.st_tok["kiT"]])
            maskadd = self.sb(st, "maskadd", [128, 4, 1024], BF16)
            t_ma = S.tok("maskadd")
            self.ld(maskadd[:], i["maskadd"], [t_ma], t_ma)
            score = [self.sb(st, "score%d" % k, [128, SEQ], F32) for k in range(2)]
            t_score = S.toks("score", 2)
            m01 = [self.sb(st, "m01%d" % k, [128, SEQ], BF16) for k in range(2)]
            t_m01 = S.toks("m01", 2)
            mT = self.sb(st, "mT", [128, NKB, 128], BF16)
            t_mT = S.tok("mT")
            NQ = 3
            qTb = [self.sb(st, "qTb%d" % k, [128, NH, 128], BF16) for k in range(NQ)]
            t_qTb = S.toks("qTb", NQ)
            qiTb = [self.sb(st, "qiTb%d" % k, [128, 16, 128], BF16) for k in range(2)]
            wbdb = [self.sb(st, "wbdb%d" % k, [128, 16, 128], BF16) for k in range(2)]
            t_qiTb, t_wbdb = S.toks("qiTb", 2), S.toks("wbdb", 2)
            NRL = 6
            rl = [self.sb(st, "rl%d" % k, [128, 512], BF16) for k in range(NRL)]
            t_rl = S.toks("rl", NRL)
            NPT = 6
            pT = [self.sb(st, "pT%d" % k, [128, 512], BF16) for k in range(NPT)]
            t_pT = S.toks("pT", NPT)
            NKV = 3
            kTc = [self.sb(st, "kTc%d" % k, [128, NG, 512], BF16) for k in range(NKV)]
            Vc = [self.sb(st, "Vc%d" % k, [128, 4, 256], BF16) for k in range(NKV)]
            t_kTc, t_Vc = S.toks("kTc", NKV), S.toks("Vc", NKV)
            bis = [self.sb(st, "bis%d" % k, [128, 16], F32) for k in range(2)]
            t_bis = S.toks("bis", 2)
            dtab = [self.sb(st, "dtab%d" % k, [128, NB], F32) for k in range(2)]
            dtab2 = [self.sb(st, "dtab2%d" % k, [128, NB], F32) for k in range(2)]
            t_dtab = S.toks("dtab", 2)
            pow2 = self.sb(st, "pow2", [128, NB], F32)
            t_pow2 = S.tok("pow2")
            for k in range(NB):
                S.op("pool", lambda e, k=k: e.memset(pow2[:, k:k + 1], float(2.0 ** -(k + 2))), writes=[t_pow2])
            tmpf = self.sb(st, "tmpf", [128, 512], F32)
            t_tmpf = S.tok("tmpf")
            ast = [self.sb(st, "ast%d" % k, [128, NH, 128], BF16) for k in range(2)]
            t_ast = S.toks("ast", 2)
            rec = [self.sb(st, "rec%d" % k, [128, 512], F32) for k in range(2)]
            t_rec = S.toks("rec", 2)
            NW = 5
            pw = [self.ps(st, "pw%d" % k, [128, 512], F32) for k in range(NW)]
            t_pw = S.toks("pw", NW)
            sc = self.ps(st, "sc", [128, 512], F32)
            t_sc = S.tok("sc")
            og1 = self.ps(st, "og", [128, 512], F32)
            sm1 = self.ps(st, "sm", [128, 512], F32)
            t_og1, t_sm1 = S.tok("og"), S.tok("sm")
            DI, DA = 3, 2
            cnt = {"w": 0, "r": 0, "p": 0, "kv": 0}
            vkb = s["v"].rearrange("(n p) c -> p n c", p=128)

            def qinfo(n):
                j, bi = n // 4, n % 4
                return j, bi, (2 * j + 2), (2 * j + 2) * 512

            def gen_I(n):
                j, bi, nkc, nk = qinfo(n)
                b = n % 2
                self.ld(qiTb[b][:], s["qiT"][n], [t_qiTb[b]], t_qiTb[b], reads=[self.st_tok["qiT"]])
                self.ld(wbdb[b][:], s["wbd"][n], [t_wbdb[b]], t_wbdb[b], reads=[self.st_tok["wbd"]])
                items = [(kc, gg, 0) for kc in range(nkc) for gg in range(16)]
                slot = {}
                for idx in range(len(items) + DI):
                    if idx < len(items):
                        kc, ii, e_ = items[idx]
                        w = cnt["w"] % NW
                        cnt["w"] += 1
                        r = cnt["r"] % NRL
                        cnt["r"] += 1
                        slot[idx] = r
                        self.mm(pw[w][:], qiTb[b][:, ii, :], kiT[:, kc * 512:(kc + 1) * 512], True, True,
                                [t_qiTb[b], t_kiT], [t_pw[w]])
                        self.act(rl[r][:], pw[w][:], AF.Relu, [t_pw[w]], [t_rl[r]])
                    k2 = idx - DI
                    if k2 >= 0:
                        kc, ii, e_ = items[k2]
                        r = slot.pop(k2)
                        n_in = ii
                        self.mm(sc[:], wbdb[b][:, ii, :], rl[r][:], n_in == 0, n_in == 15, [t_wbdb[b], t_rl[r]], [t_sc])
                        if n_in == 15:
                            dst = score[b][:, kc * 512:(kc + 1) * 512]
                            if kc >= 2 * j:
                                mk = maskadd[:, bi, (kc - 2 * j) * 512:(kc - 2 * j + 1) * 512]
                                self.tt("dve", dst, sc[:], mk, ALU.add, [t_sc, t_ma], [t_score[b]])
                                self.tt("dve", tmpf[:], sc[:], mk, ALU.subtract, [t_sc, t_ma], [t_tmpf])
                                S.op("dve", lambda e, c=3 + (kc - 2 * j), b=b: e.tensor_reduce(out=bis[b][:, c:c + 1], in_=tmpf[:], axis=AX.X, op=ALU.min),
                                     reads=[t_tmpf], writes=[t_bis[b]])
                            else:
                                self.cp("act", dst, sc[:], [t_sc], [t_score[b]])
                    if idx % 2 == 1:
                        yield

            def gen_B(n):
                j, bi, nkc, nk = qinfo(n)
                b = n % 2
                q3 = n % NQ
                B_, tb = bis[b], t_bis[b]
                self.ld(qTb[q3][:], s["qT"][n], [t_qTb[q3]], t_qTb[q3], reads=[self.st_tok["qT"]])
                S.op("dve", lambda e: e.tensor_reduce(out=B_[:, 0:1], in_=score[b][:, 0:nk], axis=AX.X, op=ALU.max),
                     reads=[t_score[b]], writes=[tb])
                if j > 0:
                    S.op("dve", lambda e: e.tensor_reduce(out=B_[:, 2:3], in_=score[b][:, 0:2 * j * 512], axis=AX.X, op=ALU.min),
                         reads=[t_score[b]], writes=[tb])
                    S.op("dve", lambda e: e.tensor_reduce(out=B_[:, 1:2], in_=B_[:, 2:5], axis=AX.X, op=ALU.min), reads=[tb], writes=[tb])
                else:
                    S.op("dve", lambda e: e.tensor_reduce(out=B_[:, 1:2], in_=B_[:, 3:5], axis=AX.X, op=ALU.min), reads=[tb], writes=[tb])
                yield
                self.tt("dve", B_[:, 5:6], B_[:, 0:1], B_[:, 1:2], ALU.subtract, [tb], [tb])
                self.ts("dve", B_[:, 5:6], B_[:, 5:6], 1.02, 2e-6, ALU.mult, ALU.add, [tb], [tb])
                self.tt("dve", B_[:, 6:7], B_[:, 0:1], B_[:, 1:2], ALU.add, [tb], [tb])
                self.ts("dve", B_[:, 6:7], B_[:, 6:7], 0.5, None, ALU.mult, None, [tb], [tb])
                self.ts("dve", dtab[b][:], pow2[:], B_[:, 5:6], None, ALU.mult, None, [tb, t_pow2], [t_dtab[b]])
                self.ts("dve", dtab2[b][:], dtab[b][:], 2.0, None, ALU.mult, None, [t_dtab[b]], [t_dtab[b]])
                yield
                for it in range(NB):
                    self.ts("dve", m01[b][:, 0:nk], score[b][:, 0:nk], B_[:, 6:7], None, ALU.is_ge, ALU.add, [t_score[b], tb], [t_m01[b], tb],
                            accum_out=B_[:, 7:8])
                    self.ts("dve", B_[:, 8:9], B_[:, 7:8], float(TOPK), dtab2[b][:, it:it + 1], ALU.is_ge, ALU.mult, [tb, t_dtab[b]], [tb])
                    self.ts("dve", B_[:, 6:7], B_[:, 8:9], dtab[b][:, it:it + 1], B_[:, 6:7], ALU.subtract, ALU.add, [tb, t_dtab[b]], [tb])
                    yield
                self.ts("dve", B_[:, 9:10], B_[:, 6:7], dtab[b][:, NB - 1:NB], None, ALU.subtract, None, [tb, t_dtab[b]], [tb])
                self.ts("dve", m01[b][:, 0:nk], score[b][:, 0:nk], B_[:, 9:10], None, ALU.is_ge, None, [t_score[b], tb], [t_m01[b]])

            def gen_A(n):
                j, bi, nkc, nk = qinfo(n)
                b = n % 2
                q3 = n % NQ
                for k4 in range(nk // 512):
                    w = cnt["w"] % NW
                    cnt["w"] += 1
                    tv = pw[w].bitcast(BF16)
                    for q_ in range(4):
                        kb = k4 * 4 + q_
                        self.tr(tv[:, q_ * 128:(q_ + 1) * 128], m01[b][:, kb * 128:(kb + 1) * 128], self.ident_b[:],
                                [t_m01[b], self.t_const], [t_pw[w]])
                    self.cp("act", mT[:, k4 * 4:(k4 + 1) * 4, :].rearrange("p a q -> p (a q)"), tv[:, 0:512], [t_pw[w]], [t_mT])
                    if k4 % 2 == 1:
                        yield
                nkb = nk // 128
                for g in range(NG):
                    items = [(k4, q_) for k4 in range(nk // 512) for q_ in range(4)]
                    slot = {}
                    kvof = {}
                    for idx in range(len(items) + DA):
                        if idx < len(items):
                            k4, q_ = items[idx]
                            if q_ == 0:
                                kv = cnt["kv"] % NKV
                                cnt["kv"] += 1
                                kvof[k4] = kv
                                self.ld(kTc[kv][:, 0, :], s["kT"][:, g, k4 * 512:(k4 + 1) * 512], [t_kTc[kv]], t_kTc[kv], reads=[self.st_tok["kT"]])
                                self.ld(Vc[kv][:, :, 0:128], vkb[:, k4 * 4:(k4 + 1) * 4, g * 128:(g + 1) * 128], [t_Vc[kv]], t_Vc[kv], reads=[self.st_tok["v"]])
                            kv = kvof[k4]
                            kb = k4 * 4 + q_
                            w = cnt["w"] % NW
                            cnt["w"] += 1
                            pp = cnt["p"] % NPT
                            cnt["p"] += 1
                            slot[idx] = pp
                            self.mm(pw[w][:], kTc[kv][:, 0, q_ * 128:(q_ + 1) * 128], qTb[q3][:, g * 4:(g + 1) * 4, :], True, True,
                                    [t_kTc[kv], t_qTb[q3]], [t_pw[w]])
                            self.act(pT[pp][:], pw[w][:], AF.Exp, [t_pw[w]], [t_pT[pp]])
                            pv = pT[pp][:].rearrange("p (h q) -> p h q", h=4)
                            self.tt("pool", pv, pv, mT[:, kb, :].unsqueeze(1).to_broadcast([128, 4, 128]), ALU.mult, [t_pT[pp], t_mT], [t_pT[pp]])
                        k2 = idx - DA
                        if k2 >= 0:
                            k4, q_ = items[k2]
                            kv = kvof[k4]
                            kb = k4 * 4 + q_
                            pp = slot.pop(k2)
                            self.mm(og1[:], Vc[kv][:, q_, 0:128], pT[pp][:], kb == 0, kb == nkb - 1, [t_Vc[kv], t_pT[pp]], [t_og1])
                            self.mm(sm1[:], self.ones_b[:], pT[pp][:], kb == 0, kb == nkb - 1, [self.t_const, t_pT[pp]], [t_sm1])
                        yield
                    S.op("dve", lambda e, g=g: e.reciprocal(out=rec[g][:], in_=sm1[:]), reads=[t_sm1], writes=[t_rec[g]])
                    self.tt("dve", ast[b][:, g * 4:(g + 1) * 4, :].rearrange("p h q -> p (h q)"), og1[:], rec[g][:], ALU.mult,
                            [t_og1, t_rec[g]], [t_ast[b]])
                self.stg(s["mixT"][n, :, 0:8, :], ast[b][:], [t_ast[b]], t_ast[b], writes=[self.st_tok["mixT"]])

            def units(kind, n):
                j, bi, nkc, nk = qinfo(n)
                return {"I": nkc * 8 + 2, "B": NB + 2, "A": 2 * (nkc * 4 + DA) + max(1, nkc // 2)}[kind] + 1

            def run_parallel(gens):
                state = [[g, float(u), 0] for g, u in gens]
                while state:
                    state.sort(key=lambda x: x[2] / x[1])
                    g = state[0]
                    try:
                        next(g[0])
                        g[2] += 1
                    except StopIteration:
                        state.pop(0)

            NQB = self.NBO
            for step in range(NQB + 2):
                gens = []
                if step - 2 >= 0:
                    gens.append((gen_A(step - 2), units("A", step - 2)))
                if 0 <= step - 1 < NQB:
                    gens.append((gen_B(step - 1), units("B", step - 1)))
                if step < NQB:
                    gens.append((gen_I(step), units("I", step)))
                run_parallel(gens)

    def phase_d(self):
        S, i, s = self.S, self.i, self.s
        with ExitStack() as st:
            w_out = self.sb(st, "w_out", [128, KC, D], BF16)
            t_wout = S.toks("w_out", 4)
            wv = i["w_out"].rearrange("(k p) n -> p k n", p=128)
            for nb in range(4):
                self.ld(w_out[:, :, nb * 512:(nb + 1) * 512], wv[:, :, nb * 512:(nb + 1) * 512], [t_wout[nb]], t_wout[nb], eng="pool")
            w_rt = self.sb(st, "w_rt", [128, KC, 36], F32)
            t_wrt = S.tok("w_rt")
            self.ld(w_rt[:], i["w_rt"].rearrange("(k p) n -> p k n", p=128), [t_wrt], t_wrt)
            gt1 = self.sb(st, "gt1", [128, D], F32)
            t_gt1 = S.tok("gt1")
            self.ld(gt1[:], s["gt"][0:1, :].partition_broadcast(128), [t_gt1], t_gt1, reads=[self.st_tok["gt"]])
            epsc = self.sb(st, "epsc", [128, 1], F32)
            t_eps = S.tok("epsc")
            S.op("dve", lambda e: e.memset(epsc[:], EPS), writes=[t_eps])
            xt = [self.sb(st, "xt%d" % k, [128, D], F32) for k in range(2)]
            t_xt = S.toks("xt", 2)
            mx = [self.sb(st, "mx%d" % k, [128, KC, 128], BF16) for k in range(2)]
            t_mx = S.toks("mx", 2)
            R = {
                "ss": self.sb(st, "ss", [128, 4], F32), "t_ss": S.tok("ss"),
                "xs": self.sb(st, "xs", [128, D], F32), "t_xs": S.tok("xs"),
                "xtp": self.ps(st, "xtp", [128, D], F32), "t_xtp": S.toks("xtp", 4),
                "epsc": epsc, "t_eps": t_eps,
            }
            R["junk"], R["t_junk"] = R["xs"], R["t_xs"]
            hT = [self.sb(st, "hT%d" % k, [128, KC, 128], BF16) for k in range(2)]
            t_hT = S.toks("hT", 2)
            hTf = self.sb(st, "hTf", [128, KC, 128], F32)
            t_hTf = S.tok("hTf")
            tmp = [self.sb(st, "tmp%d" % k, [128, 512], F32) for k in range(2)]
            t_tmp = S.toks("tmp", 2)
            pj = [self.ps(st, "pj%d" % k, [128, 512], F32) for k in range(2)]
            t_pj = S.toks("pj", 2)
            prt = self.ps(st, "prt", [128, 512], F32)
            t_prt = S.tok("prt")
            rt = self.sb(st, "rt", [128, 36], F32)
            t_rt = S.tok("rt")
            rs = self.sb(st, "rs", [128, 64], F32)
            t_rs = S.tok("rs")
            SH2 = self.cols[:, 48:64]
            n = 0
            for ob in range(self.NBO):
                j, bi = ob // 4, ob % 4
                b = ob % 2
                tok0 = (2 * j) * 512 + bi * 128
                self.ld(xt[b][:], i["x_loc"][tok0:tok0 + 128, :], [t_xt[b]], t_xt[b])
                self.ld(mx[b][:], s["mixT"][ob], [t_mx[b]], t_mx[b], reads=[self.st_tok["mixT"]])
                for nb in range(4):
                    w = n % 2
                    n += 1
                    for kc in range(KC):
                        self.mm(pj[w][:], mx[b][:, kc, :], w_out[:, kc, nb * 512:(nb + 1) * 512], kc == 0, kc == KC - 1,
                                [t_mx[b], t_wout[nb]], [t_pj[w]])
                    self.tt("dve", tmp[w][:], pj[w][:], gt1[:, nb * 512:(nb + 1) * 512], ALU.mult, [t_pj[w], t_gt1], [t_tmp[w]])
                    self.tt("pool", xt[b][:, nb * 512:(nb + 1) * 512], xt[b][:, nb * 512:(nb + 1) * 512], tmp[w][:], ALU.add,
                            [t_tmp[w], t_xt[b]], [t_xt[b]])
                self.stg(s["x1"][ob * 128:(ob + 1) * 128, :], xt[b][:], [t_xt[b]], t_xt[b], writes=[self.st_tok["x1"]])
                import os as _os
                DST = int(_os.environ.get("DSTAGE", "4"))
                if DST < 2:
                    continue
                self.norm_block(xt[b], t_xt[b], self.G2, SH2, hT[b], t_hT[b], R, hTf=hTf, t_hTf=t_hTf)
                self.stg(s["h2T"][ob], hT[b][:], [t_hT[b]], t_hT[b], writes=[self.st_tok["h2T"]])
                if DST < 3:
                    continue
                for kc in range(KC):
                    self.mm(prt[:, 0:36], hTf[:, kc, :], w_rt[:, kc, :], kc == 0, kc == KC - 1, [t_hTf, t_wrt], [t_prt])
                self.cp("act", rt[:], prt[:, 0:36], [t_prt], [t_rt])
                if DST < 4:
                    continue
                dv = lambda *a, **k: None
                X = lambda a, b_: rs[:, a:b_]
                S.op("dve", lambda e: e.tensor_reduce(out=X(0, 1), in_=rt[:, 0:4], axis=AX.X, op=ALU.max), reads=[t_rt], writes=[t_rs])
                self.ts("dve", X(4, 8), rt[:, 0:4], X(0, 1), None, ALU.is_equal, None, [t_rt, t_rs], [t_rs])
                self.ts("dve", X(1, 2), X(0, 1), -1.0, None, ALU.mult, None, [t_rs], [t_rs])
                self.act(X(8, 12), rt[:, 0:4], AF.Exp, [t_rt, t_rs], [t_rs], bias=X(1, 2), accum_out=X(2, 3))
                S.op("dve", lambda e: e.reciprocal(out=X(3, 4), in_=X(2, 3)), reads=[t_rs], writes=[t_rs])
                self.ts("dve", X(16, 24), rt[:, 4:12], X(4, 5), None, ALU.mult, None, [t_rt, t_rs], [t_rs])
                for g in range(1, 4):
                    self.stt("dve", X(16, 24), rt[:, 4 + 8 * g:12 + 8 * g], X(4 + g, 5 + g), X(16, 24), ALU.mult, ALU.add, [t_rt, t_rs], [t_rs])
                S.op("dve", lambda e: e.tensor_reduce(out=X(12, 13), in_=X(16, 24), axis=AX.X, op=ALU.max), reads=[t_rs], writes=[t_rs])
                self.ts("dve", X(24, 32), X(16, 24), X(12, 13), None, ALU.is_equal, None, [t_rs], [t_rs])
                self.stt("dve", X(32, 40), X(24, 32), NEG, X(16, 24), ALU.mult, ALU.add, [t_rs], [t_rs])
                S.op("dve", lambda e: e.tensor_reduce(out=X(13, 14), in_=X(32, 40), axis=AX.X, op=ALU.max), reads=[t_rs], writes=[t_rs])
                self.ts("dve", X(40, 48), X(32, 40), X(13, 14), None, ALU.is_equal, None, [t_rs], [t_rs])
                self.tt("dve", X(14, 15), X(13, 14), X(12, 13), ALU.subtract, [t_rs], [t_rs])
                self.act(X(15, 16), X(14, 15), AF.Exp, [t_rs], [t_rs])
                self.ts("dve", X(15, 16), X(15, 16), 1.0, None, ALU.add, None, [t_rs], [t_rs])
                S.op("dve", lambda e: e.reciprocal(out=X(48, 49), in_=X(15, 16)), reads=[t_rs], writes=[t_rs])
                self.tt("dve", X(49, 50), X(48, 49), X(3, 4), ALU.mult, [t_rs], [t_rs])
                self.tt("dve", X(50, 51), X(3, 4), X(49, 50), ALU.subtract, [t_rs], [t_rs])
                self.ts("dve", X(52, 60), X(24, 32), X(49, 50), None, ALU.mult, None, [t_rs], [t_rs])
                self.stt("dve", X(52, 60), X(40, 48), X(50, 51), X(52, 60), ALU.mult, ALU.add, [t_rs], [t_rs])
                for g in range(4):
                    self.ts("dve", self.comb[:, ob, g * 8:(g + 1) * 8], X(52, 60), X(4 + g, 5 + g), None, ALU.mult, None, [t_rs], [self.t_comb])

    def phase_e(self):
        S, i, s = self.S, self.i, self.s
        with ExitStack() as st:
            gt2 = self.sb(st, "gt2", [128, D], F32)
            t_gt2 = S.tok("gt2")
            self.ld(gt2[:], s["gt"][1:2, :].partition_broadcast(128), [t_gt2], t_gt2, reads=[self.st_tok["gt"]])
            h2 = self.sb(st, "h2", [128, KC, 512], BF16)
            t_h2 = S.tok("h2")
            t_h2p = S.toks("h2p", 4)
            yacc = self.sb(st, "yacc", [128, 4, D], F32)
            t_yacc = S.toks("yacc", 4)
            w1b = [self.sb(st, "w1b%d" % k, [128, KC, DEXP], BF16) for k in range(2)]
            w3b = [self.sb(st, "w3b%d" % k, [128, KC, DEXP], BF16) for k in range(2)]
            w2b = [self.sb(st, "w2b%d" % k, [128, 4, D], BF16) for k in range(2)]
            t_w1, t_w3, t_w2 = S.toks("w1b", 2), S.toks("w3b", 2), S.toks("w2b", 2)
            aT = self.sb(st, "aT", [128, 4, 512], BF16)
            t_aT = S.toks("aT", 4)
            s1 = [self.sb(st, "s1%d" % k, [128, 512], BF16) for k in range(2)]
            t_s1 = S.toks("s1", 2)
            x1t = [self.sb(st, "x1t%d" % k, [128, D], F32) for k in range(2)]
            t_x1t = S.toks("x1t", 2)
            ph1 = [self.ps(st, "ph1%d" % k, [128, 512], F32) for k in range(2)]
            ph3 = [self.ps(st, "ph3%d" % k, [128, 512], F32) for k in range(2)]
            py = [self.ps(st, "py%d" % k, [128, 512], F32) for k in range(2)]
            t_ph1, t_ph3, t_py = S.toks("ph1", 2), S.toks("ph3", 2), S.toks("py", 2)
            n_h = 0
            n_y = 0
            n_e = 0
            n_x = 0
            for j in range(self.NOWN):
                for bi in range(4):
                    self.ld(h2[:, :, bi * 128:(bi + 1) * 128], s["h2T"][4 * j + bi], [t_h2], t_h2, reads=[self.st_tok["h2T"]])
                for e_ in range(NEXP):
                    wb = n_e % 2
                    n_e += 1
                    self.ld(w1b[wb][:], i["w1"][e_].rearrange("(k p) f -> p k f", p=128), [t_w1[wb]], t_w1[wb], eng="pool")
                    self.ld(w3b[wb][:], i["w3"][e_].rearrange("(k p) f -> p k f", p=128), [t_w3[wb]], t_w3[wb], eng="pool")
                    self.ld(w2b[wb][:], i["w2"][e_].rearrange("(k p) n -> p k n", p=128), [t_w2[wb]], t_w2[wb], eng="pool")
                    for fc in range(4):
                        hb = n_h % 2
                        n_h += 1
                        for kc in range(KC):
                            self.mm(ph1[hb][:], w1b[wb][:, kc, fc * 128:(fc + 1) * 128], h2[:, kc, :], kc == 0, kc == KC - 1,
                                    [t_w1[wb], t_h2], [t_ph1[hb]])
                        for kc in range(KC):
                            self.mm(ph3[hb][:], w3b[wb][:, kc, fc * 128:(fc + 1) * 128], h2[:, kc, :], kc == 0, kc == KC - 1,
                                    [t_w3[wb], t_h2], [t_ph3[hb]])
                        self.act(s1[hb][:], ph1[hb][:], AF.Silu, [t_ph1[hb]], [t_s1[hb]])
                        self.tt("dve", aT[:, fc, :], s1[hb][:], ph3[hb][:], ALU.mult, [t_s1[hb], t_ph3[hb]], [t_aT[fc]])
                    for bi in range(4):
                        cw = self.comb[:, 4 * j + bi, e_:e_ + 1]
                        for nb in range(4):
                            yb = n_y % 2
                            n_y += 1
                            for fc in range(4):
                                self.mm(py[yb][:], aT[:, fc, bi * 128:(bi + 1) * 128], w2b[wb][:, fc, nb * 512:(nb + 1) * 512], fc == 0, fc == 3,
                                        [t_aT[fc], t_w2[wb]], [t_py[yb]])
                            ys = yacc[:, bi, nb * 512:(nb + 1) * 512]
                            if e_ == 0:
                                self.ts("dve", ys, py[yb][:], cw, None, ALU.mult, None, [t_py[yb], self.t_comb], [t_yacc[bi]])
                            else:
                                self.stt("dve", ys, py[yb][:], cw, ys, ALU.mult, ALU.add, [t_py[yb], self.t_comb, t_yacc[bi]], [t_yacc[bi]])
                for bi in range(4):
                    xb = n_x % 2
                    n_x += 1
                    ob = 4 * j + bi
                    self.ld(x1t[xb][:], s["x1"][ob * 128:(ob + 1) * 128, :], [t_x1t[xb]], t_x1t[xb], reads=[self.st_tok["x1"]])
                    self.tt("pool", yacc[:, bi, :], yacc[:, bi, :], gt2[:], ALU.mult, [t_yacc[bi], t_gt2], [t_yacc[bi]])
                    self.tt("pool", x1t[xb][:], x1t[xb][:], yacc[:, bi, :], ALU.add, [t_x1t[xb], t_yacc[bi]], [t_x1t[xb]])
                    self.stg(self.out[ob * 128:(ob + 1) * 128, :], x1t[xb][:], [t_x1t[xb]], t_x1t[xb])


def _bf(a):
    return np.ascontiguousarray(a).astype(ml_dtypes.bfloat16)


def _rope_table(pos):
    pos = pos.astype(np.float32)
    out = np.zeros((pos.shape[0], 48), np.float32)
    for (rd, c0) in ((32, 0), (16, 32)):
        half = rd // 2
        inv = np.float32(500000.0) ** (-(np.arange(half, dtype=np.float32) * np.float32(2.0)) / np.float32(rd))
        ang = (pos[:, None] * inv[None, :]).astype(np.float32)
        out[:, c0:c0 + half] = np.cos(ang)
        out[:, c0 + half:c0 + 2 * half] = np.sin(ang)
    return out


def _pool_mats(NOWN, p):
    wins = (2, 4, 8, 16)
    M = np.zeros((128, 12 + 4 * NOWN, 128), np.float32)
    src = np.arange(128)[:, None]
    dst = np.arange(128)[None, :]
    for g, w in enumerate(wins):
        cur = ((src <= dst) & (src >= dst - w + 1)).astype(np.float32) / w - (src == dst)
        prev = ((src - 128) >= (dst - w + 1)).astype(np.float32) / w
        M[:, g, :] = cur
        M[:, 4 + g, :] = prev
        if p == 0:
            cnt = np.minimum(dst + 1, w).astype(np.float32)
            M[:, 8 + g, :] = ((src <= dst) & (src >= dst - w + 1)).astype(np.float32) / cnt - (src == dst)
        else:
            M[:, 8 + g, :] = cur
        for j in range(NOWN):
            if p == 0 and j == 0:
                continue
            r = src - 16 * j
            valid = (r >= 0) & (r < 16) & ((r - 16) >= (dst - w + 1))
            M[:, 12 + 4 * j + g, :] = valid.astype(np.float32) / w
    return _bf(M)


def _consts(p, NOWN):
    c = {}
    c["ident_f"] = np.eye(128, dtype=np.float32)
    c["ident_b"] = _bf(np.eye(128, dtype=np.float32))
    rr = np.zeros((16, 128), np.float32)
    mi = np.zeros((128, 16, 128), np.float32)
    for e in range(2):
        for cc in range(8):
            for t in range(8):
                r = e * 64 + cc * 8 + t
                rr[2 * cc + e, r] = 1.0
                for gg in range(16):
                    mi[r, gg, 8 * gg + t] = 1.0
    c["rrep"] = _bf(rr)
    c["maski"] = _bf(mi)
    ma = np.zeros((128, 4, 1024), np.float32)
    kk = np.arange(512)[None, :]
    for bi in range(4):
        qq = bi * 128 + np.arange(128)[:, None]
        ma[:, bi, 0:512] = np.where(kk <= qq, 0.0, NEG)
        ma[:, bi, 512:1024] = 0.0 if p == 1 else NEG
    c["maskadd"] = _bf(ma)
    c["poolA"] = _pool_mats(NOWN, p)
    return c


def _col(v, n):
    return np.ascontiguousarray(np.asarray(v, np.float32).reshape(n, 128).T)


def make_in_maps(inputs):
    x = np.asarray(inputs["x"], np.float32)
    B, SEQ, _ = x.shape
    NL = SEQ // 512
    NOWN = NL // 2
    shared = {
        "w_ada": np.ascontiguousarray(inputs["w_ada"][0], dtype=np.float32),
        "b_ada": np.ascontiguousarray(inputs["b_ada"][0], dtype=np.float32).reshape(1, -1),
        "g1col": _col(inputs["norm1_g"][0], KC),
        "g2col": _col(inputs["norm2_g"][0], KC),
        "w_in": np.ascontiguousarray(inputs["w_in"][0], dtype=np.float32),
        "qg": np.asarray(inputs["q_norm_g"][0], np.float32).reshape(1, 128),
        "kg": np.asarray(inputs["k_norm_g"][0], np.float32).reshape(1, 128),
        "w_pool": np.ascontiguousarray(inputs["w_pool"][0], dtype=np.float32),
        "pscol": _col(np.asarray(inputs["pool_scale"][0]).reshape(-1), 8),
        "w_out": np.ascontiguousarray(inputs["w_out"][0], dtype=np.float32),
        "w_rt": np.ascontiguousarray(np.concatenate([inputs["w_grp"][0], inputs["w_exp"][0]], axis=1), dtype=np.float32),
        "w1": np.ascontiguousarray(inputs["w1"][0], dtype=np.float32),
        "w3": np.ascontiguousarray(inputs["w3"][0], dtype=np.float32),
        "w2": np.ascontiguousarray(inputs["w2"][0], dtype=np.float32),
    }
    consts = [_consts(p, NOWN) for p in range(2)]
    maps = []
    for b in range(B):
        for p in range(2):
            order = [(s ^ 1) if p == 1 else s for s in range(NL)]
            xl = np.concatenate([x[b, g * 512:(g + 1) * 512] for g in order], axis=0)
            pos = np.concatenate([np.arange(g * 512, (g + 1) * 512) for g in order])
            halo = np.zeros((128, D), np.float32)
            for j in range(NOWN):
                g = order[2 * j]
                if g > 0:
                    halo[16 * j:16 * j + 16] = x[b, g * 512 - 16:g * 512]
            m = dict(shared)
            m.update(consts[p])
            m["x_loc"] = np.ascontiguousarray(xl)
            m["x_halo"] = halo
            m["cvec"] = _col(inputs["c"][b], KC)
            m["rope"] = _rope_table(pos)
            maps.append(m)
    return maps, B, SEQ


_CACHE = {}


def kernel(**inputs):
    maps, B, SEQ = make_in_maps(inputs)
    NL = SEQ // 512
    if SEQ not in _CACHE:
        _CACHE[SEQ] = Prog(SEQ).build()
    nc = _CACHE[SEQ]
    res = run_bass_kernel_spmd(nc, maps, core_ids=list(range(2 * B)))
    out = np.zeros((B, SEQ, D), np.float32)
    for b in range(B):
        for p in range(2):
            o = res.results[2 * b + p]["out"]
            for j in range(NL // 2):
                g = 2 * j + p
                out[b, g * 512:(g + 1) * 512] = o[j * 512:(j + 1) * 512]
    return out
```

```python
from contextlib import ExitStack
import numpy as np
import ml_dtypes
import concourse.bass as bass
import concourse.mybir as mybir
from concourse.bass_utils import run_bass_kernel_spmd

F32 = mybir.dt.float32
BF16 = mybir.dt.bfloat16
AF = mybir.ActivationFunctionType
ALU = mybir.AluOpType
AX = mybir.AxisListType

D = 2048
KC = 16
NH = 8
NG = 2
HD = 128
IH = 16
IDM = 64
PW = 1024
PROJ = 3664
NEXP = 32
DEXP = 512
EPS = 1e-6
NEG = -1.0e30
TOPK = 256
N_BISECT = 18
C_Q, C_K, C_V, C_QI, C_KI, C_WI, C_P = 0, 1024, 1280, 1536, 2560, 2624, 2640


class Tok:
    __slots__ = ("name", "w", "r", "dsem")

    def __init__(self, name):
        self.name = name
        self.w = None
        self.r = {}
        self.dsem = None


class Sched:
    ENG = ("pe", "act", "dve", "pool", "sp")

    def __init__(self, nc, stack):
        self.nc = nc
        self.stack = stack
        self.sems = {}
        self.count = {}
        self.waited = {e: {} for e in self.ENG}
        self.prog = {e: [] for e in self.ENG}
        for e in self.ENG:
            self._mksem(e)
        self.free_dsems = []
        self.n_ops = 0

    def _mksem(self, key):
        s = self.stack.enter_context(self.nc.semaphore("s_" + str(key)))
        self.sems[key] = s
        self.count[key] = 0
        return s

    def tok(self, name):
        return Tok(name)

    def toks(self, name, n):
        return [Tok("%s%d" % (name, i)) for i in range(n)]

    def _need(self, eng, ev, waits, raw=False):
        if ev is None:
            return
        k, v = ev
        if k == eng and eng == "pe":
            return
        if self.waited[eng].get(k, 0) >= v:
            return
        if waits.get(k, 0) < v:
            waits[k] = v

    def _deps(self, eng, reads, writes):
        waits = {}
        for t in reads:
            self._need(eng, t.w, waits, raw=True)
        for t in writes:
            self._need(eng, t.w, waits)
            for k, v in t.r.items():
                self._need(eng, (k, v), waits)
        for k, v in waits.items():
            self.waited[eng][k] = v
        return waits

    def op(self, eng, fn, reads=(), writes=()):
        waits = self._deps(eng, reads, writes)
        self.count[eng] += 1
        v = self.count[eng]
        self.prog[eng].append((fn, waits, (eng, 1)))
        for t in reads:
            t.r[eng] = v
        for t in writes:
            t.w = (eng, v)
            t.r = {}
        self.n_ops += 1
        return v

    def dma(self, eng, fn, reads=(), writes=(), owner=None):
        assert owner is not None
        if owner.dsem is None:
            owner.dsem = "d%d" % len(self.sems)
            self._mksem(owner.dsem)
        k = owner.dsem
        waits = self._deps(eng, reads, writes)
        if self.count[k] > 0 and self.waited[eng].get(k, 0) < self.count[k]:
            waits[k] = self.count[k]
            self.waited[eng][k] = self.count[k]
        self.count[k] += 16
        v = self.count[k]
        self.prog[eng].append((fn, waits, (k, 16)))
        for t in reads:
            t.r[k] = v
        for t in writes:
            t.w = (k, v)
            t.r = {}
        self.n_ops += 1
        return (k, v)

    def barrier(self, engines=None):
        engines = engines or self.ENG
        for e in engines:
            waits = {}
            for k, c in self.count.items():
                if k == e or c == 0:
                    continue
                if self.waited[e].get(k, 0) < c:
                    waits[k] = c
                    self.waited[e][k] = c
            if waits:
                self.prog[e].append((None, waits, None))

    def emit(self, block):
        sems = self.sems

        def run(e_obj, prog):
            for fn, waits, inc in prog:
                for k, v in waits.items():
                    e_obj.wait_ge(sems[k], v)
                if fn is None:
                    continue
                ins = fn(e_obj)
                ins.then_inc(sems[inc[0]], inc[1])

        @block.tensor
        def _(e):
            run(e, self.prog["pe"])

        @block.scalar
        def _(e):
            run(e, self.prog["act"])

        @block.vector
        def _(e):
            run(e, self.prog["dve"])

        @block.gpsimd
        def _(e):
            run(e, self.prog["pool"])

        @block.sync
        def _(e):
            run(e, self.prog["sp"])


class Prog:
    def __init__(self, SEQ, debug=False, stop_after=None):
        self.SEQ = SEQ
        self.NL = SEQ // 512
        self.NOWN = self.NL // 2
        self.NBO = self.NOWN * 4
        self.NBA = SEQ // 128
        self.debug = debug
        self.stop_after = stop_after
        self.nc = bass.Bass("TRN2", target_bir_lowering=False)

    def din(self, name, shape, dt=F32):
        return self.nc.dram_tensor(name, list(shape), dt, kind="ExternalInput").ap()

    def dscr(self, name, shape, dt):
        kind = "ExternalOutput" if self.debug else "Internal"
        return self.nc.dram_tensor(name, list(shape), dt, kind=kind).ap()

    def sb(self, st, name, shape, dt):
        self._uid = getattr(self, "_uid", 0) + 1
        return st.enter_context(self.nc.sbuf_tensor("sb%d_%s" % (self._uid, name), list(shape), dt))

    def ps(self, st, name, shape, dt):
        self._uid = getattr(self, "_uid", 0) + 1
        return st.enter_context(self.nc.psum_tensor("ps%d_%s" % (self._uid, name), list(shape), dt))

    def mm(self, out, lhsT, rhs, start, stop, reads, writes):
        self.S.op("pe", lambda e: e.matmul(out, lhsT=lhsT, rhs=rhs, start=start, stop=stop), reads=reads, writes=writes)

    def tr(self, out, in_, ident, reads, writes):
        self.S.op("pe", lambda e: e.transpose(out, in_, ident), reads=reads, writes=writes)

    def act(self, out, in_, func, reads, writes, bias=None, scale=1.0, accum_out=None, eng="act"):
        kw = {}
        if bias is not None:
            kw["bias"] = bias
        if accum_out is not None:
            kw["accum_out"] = accum_out
        self.S.op(eng, lambda e: e.activation(out=out, in_=in_, func=func, scale=scale, **kw), reads=reads, writes=writes)

    def ts(self, eng, out, in0, s1, s2, op0, op1, reads, writes, accum_out=None):
        kw = {}
        if accum_out is not None:
            kw["accum_out"] = accum_out
        if op1 is None:
            self.S.op(eng, lambda e: e.tensor_scalar(out=out, in0=in0, scalar1=s1, scalar2=None, op0=op0, **kw), reads=reads, writes=writes)
        else:
            self.S.op(eng, lambda e: e.tensor_scalar(out=out, in0=in0, scalar1=s1, scalar2=s2, op0=op0, op1=op1, **kw), reads=reads, writes=writes)

    def tt(self, eng, out, in0, in1, op, reads, writes):
        self.S.op(eng, lambda e: e.tensor_tensor(out=out, in0=in0, in1=in1, op=op), reads=reads, writes=writes)

    def stt(self, eng, out, in0, scalar, in1, op0, op1, reads, writes):
        self.S.op(eng, lambda e: e.scalar_tensor_tensor(out=out, in0=in0, scalar=scalar, in1=in1, op0=op0, op1=op1), reads=reads, writes=writes)

    def cp(self, eng, out, in_, reads, writes):
        if eng == "act":
            self.S.op(eng, lambda e: e.activation(out=out, in_=in_, func=AF.Copy), reads=reads, writes=writes)
        else:
            self.S.op(eng, lambda e: e.tensor_copy(out=out, in_=in_), reads=reads, writes=writes)

    def ld(self, out, in_, writes, owner, reads=(), eng="sp", **kw):
        return self.S.dma(eng, lambda e: e.dma_start(out=out, in_=in_, **kw), reads=reads, writes=writes, owner=owner)

    def stg(self, out, in_, reads, owner, writes=(), eng="sp"):
        return self.S.dma(eng, lambda e: e.dma_start(out=out, in_=in_), reads=reads, writes=writes, owner=owner)

    def declare(self):
        SEQ, NBO = self.SEQ, self.NBO
        i = {}
        i["x_loc"] = self.din("x_loc", [SEQ, D])
        i["x_halo"] = self.din("x_halo", [128, D])
        i["cvec"] = self.din("cvec", [128, KC])
        i["rope"] = self.din("rope", [SEQ, 48])
        i["maskadd"] = self.din("maskadd", [128, 4, 1024], BF16)
        i["poolA"] = self.din("poolA", [128, 12 + 4 * self.NOWN, 128], BF16)
        i["ident_f"] = self.din("ident_f", [128, 128])
        i["ident_b"] = self.din("ident_b", [128, 128], BF16)
        i["rrep"] = self.din("rrep", [16, 128], BF16)
        i["maski"] = self.din("maski", [128, 16, 128], BF16)
        i["w_ada"] = self.din("w_ada", [D, 6 * D])
        i["b_ada"] = self.din("b_ada", [1, 6 * D])
        i["g1col"] = self.din("g1col", [128, KC])
        i["g2col"] = self.din("g2col", [128, KC])
        i["w_in"] = self.din("w_in", [D, PROJ])
        i["qg"] = self.din("qg", [1, 128])
        i["kg"] = self.din("kg", [1, 128])
        i["w_pool"] = self.din("w_pool", [4, 256, 256])
        i["pscol"] = self.din("pscol", [128, 8])
        i["w_out"] = self.din("w_out", [D, D])
        i["w_rt"] = self.din("w_rt", [D, 36])
        i["w1"] = self.din("w1", [NEXP, D, DEXP])
        i["w3"] = self.din("w3", [NEXP, D, DEXP])
        i["w2"] = self.din("w2", [NEXP, DEXP, D])
        self.i = i
        self.out = self.nc.dram_tensor("out", [self.NOWN * 512, D], F32, kind="ExternalOutput").ap()
        s = {}
        s["kT"] = self.dscr("kT_s", [128, NG, SEQ], BF16)
        s["v"] = self.dscr("v_s", [SEQ, 256], BF16)
        s["kiT"] = self.dscr("kiT_s", [128, SEQ], BF16)
        s["qT"] = self.dscr("qT_s", [NBO, 128, NH, 128], BF16)
        s["qiT"] = self.dscr("qiT_s", [NBO, 128, 16, 128], BF16)
        s["wbd"] = self.dscr("wbd_s", [NBO, 128, 16, 128], BF16)
        s["mixT"] = self.dscr("mixT_s", [NBO, 128, KC, 128], BF16)
        s["x1"] = self.dscr("x1_s", [NBO * 128, D], F32)
        s["h2T"] = self.dscr("h2T_s", [NBO, 128, KC, 128], BF16)
        s["gt"] = self.dscr("gt_s", [2, D], F32)
        self.s = s
        self.st_tok = {k: Tok("scr_" + k) for k in s}

    def build(self):
        nc = self.nc
        self.declare()
        with ExitStack() as top:
            self.S = S = Sched(nc, top)
            self.cols = self.sb(top, "cols", [128, 96], F32)
            self.t_cols = S.tok("cols")
            self.G1 = self.sb(top, "G1", [128, KC], F32)
            self.G2 = self.sb(top, "G2", [128, KC], F32)
            self.t_G = S.tok("G")
            self.ident_f = self.sb(top, "ident_f", [128, 128], F32)
            self.ident_b = self.sb(top, "ident_b", [128, 128], BF16)
            self.ones_f = self.sb(top, "ones_f", [128, 128], F32)
            self.ones_b = self.sb(top, "ones_b", [128, 128], BF16)
            self.t_const = S.tok("const")
            tcf, tcb = S.tok("cf"), S.tok("cb")
            self.ld(self.ident_f[:], self.i["ident_f"], [tcf], tcf)
            self.ld(self.ident_b[:], self.i["ident_b"], [tcb], tcb)
            S.op("dve", lambda e: e.memset(self.ones_f[:], 1.0), reads=[tcf, tcb], writes=[self.t_const])
            S.op("dve", lambda e: e.memset(self.ones_b[:], 1.0), writes=[self.t_const])
            self.comb = self.sb(top, "comb", [128, self.NBO, NEXP], F32)
            self.t_comb = S.tok("comb")

            self.phase_a()
            S.barrier()
            if self.stop_after != "a":
                self.phase_b()
                S.barrier()
            if self.stop_after not in ("a", "b"):
                self.phase_c()
                S.barrier()
            if self.stop_after not in ("a", "b", "c"):
                self.phase_d()
                S.barrier()
            if self.stop_after not in ("a", "b", "c", "d"):
                self.phase_e()
            if self.stop_after is not None:
                tdb = S.tok("dbg")
                self.stg(self.out[0:128, 0:96], self.cols[:], [self.t_cols], tdb)
            S.barrier()
            with nc.Block() as block:
                S.emit(block)
        return nc

    def phase_a(self):
        S = self.S
        with ExitStack() as st:
            cv = self.sb(st, "cv", [128, KC], F32)
            sc = self.sb(st, "sc", [128, KC], F32)
            crep = self.sb(st, "crep", [128, KC, 128], F32)
            t_cv, t_sc, t_crep = S.tok("cv"), S.tok("sc"), S.tok("crep")
            wa = [self.sb(st, "wa%d" % k, [128, KC, 512], F32) for k in range(2)]
            t_wa = S.toks("wa", 2)
            ba = self.sb(st, "ba", [1, 6 * D], F32)
            t_ba = S.tok("ba")
            ada = self.sb(st, "ada", [128, 6 * D], F32)
            t_ada = S.tok("ada")
            g1c = self.sb(st, "g1c", [128, KC], F32)
            g2c = self.sb(st, "g2c", [128, KC], F32)
            t_g1c, t_g2c = S.tok("g1c"), S.tok("g2c")
            pa = [self.ps(st, "pa%d" % k, [128, 512], F32) for k in range(2)]
            t_pa = S.toks("pa", 2)

            self.ld(cv[:], self.i["cvec"], [t_cv], t_cv)
            self.ld(ba[:], self.i["b_ada"], [t_ba], t_ba)
            self.ld(g1c[:], self.i["g1col"], [t_g1c], t_g1c)
            self.ld(g2c[:], self.i["g2col"], [t_g2c], t_g2c)
            self.act(sc[:], cv[:], AF.Silu, [t_cv], [t_sc])
            self.cp("dve", crep[:], sc[:].unsqueeze(2).to_broadcast([128, KC, 128]), [t_sc], [t_crep])
            wv = self.i["w_ada"].rearrange("(k p) n -> p k n", p=128)
            for nb in range(24):
                b = nb % 2
                self.ld(wa[b][:], wv[:, :, nb * 512:(nb + 1) * 512], [t_wa[b]], t_wa[b])
                for kc in range(KC):
                    self.mm(pa[b][:], crep[:, kc, :], wa[b][:, kc, :], kc == 0, False, [t_crep, t_wa[b]], [t_pa[b]])
                self.mm(pa[b][:], self.ones_f[0:1, :], ba[0:1, nb * 512:(nb + 1) * 512], False, True,
                        [self.t_const, t_ba], [t_pa[b]])
                self.cp("act" if nb % 2 else "dve", ada[:, nb * 512:(nb + 1) * 512], pa[b][:], [t_pa[b]], [t_ada])
            for hf in range(2):
                tmpv = wa[hf][:].rearrange("p k n -> p (k n)")[:, 0:48 * 128].rearrange("p (j i) -> p j i", i=128)
                self.tt("dve", tmpv, ada[:, hf * 6144:(hf + 1) * 6144].rearrange("p (j i) -> p j i", i=128),
                        self.ident_f[:].unsqueeze(1).to_broadcast([128, 48, 128]), ALU.mult,
                        [t_ada, self.t_const], [t_wa[hf]])
                S.op("dve", lambda e, tmpv=tmpv, hf=hf: e.tensor_reduce(out=self.cols[:, hf * 48:(hf + 1) * 48], in_=tmpv, axis=AX.X, op=ALU.add),
                     reads=[t_wa[hf]], writes=[self.t_cols])
            self.stt("dve", self.G1[:], self.cols[:, 16:32], 1.0, g1c[:], ALU.add, ALU.mult, [self.t_cols, t_g1c], [self.t_G])
            self.stt("dve", self.G2[:], self.cols[:, 64:80], 1.0, g2c[:], ALU.add, ALU.mult, [self.t_cols, t_g2c], [self.t_G])
            self.stg(self.s["gt"][0:1, :], ada[0:1, 2 * D:3 * D], [t_ada], t_ada, writes=[self.st_tok["gt"]])
            self.stg(self.s["gt"][1:2, :], ada[0:1, 5 * D:6 * D], [t_ada], t_ada, writes=[self.st_tok["gt"]])

    def norm_block(self, xt, t_xt, Gc, SHc, hT, t_hT, R, hTf=None, t_hTf=None):
        self.act(R["junk"][:], xt[:], AF.Square, [t_xt], [R["t_junk"], R["t_ss"]], accum_out=R["ss"][:, 0:1])
        self.act(R["ss"][:, 1:2], R["ss"][:, 0:1], AF.Sqrt, [R["t_ss"], R["t_eps"]], [R["t_ss"]], bias=R["epsc"][:, 0:1], scale=1.0 / D)
        self.S.op("dve", lambda e: e.reciprocal(out=R["ss"][:, 2:3], in_=R["ss"][:, 1:2]), reads=[R["t_ss"]], writes=[R["t_ss"]])
        self.act(R["xs"][:], xt[:], AF.Copy, [t_xt, R["t_ss"]], [R["t_xs"]], scale=R["ss"][:, 2:3])
        for kc in range(KC):
            self.tr(R["xtp"][:, kc * 128:(kc + 1) * 128], R["xs"][:, kc * 128:(kc + 1) * 128], self.ident_f[:],
                    [R["t_xs"], self.t_const], [R["t_xtp"][kc // 4]])
        for kc in range(KC):
            if hTf is None:
                self.ts("dve", hT[:, kc, :], R["xtp"][:, kc * 128:(kc + 1) * 128], Gc[:, kc:kc + 1], SHc[:, kc:kc + 1],
                        ALU.mult, ALU.add, [R["t_xtp"][kc // 4], self.t_G, self.t_cols], [t_hT])
            else:
                self.ts("dve", hTf[:, kc, :], R["xtp"][:, kc * 128:(kc + 1) * 128], Gc[:, kc:kc + 1], SHc[:, kc:kc + 1],
                        ALU.mult, ALU.add, [R["t_xtp"][kc // 4], self.t_G, self.t_cols], [t_hTf])
        if hTf is not None:
            self.cp("act", hT[:].rearrange("p k t -> p (k t)"), hTf[:].rearrange("p k t -> p (k t)"), [t_hTf], [t_hT])

    def rope_ops(self, src, dst, H, half, cos, sin, tmp, reads, t_dst, t_tmp, eng="dve"):
        x1 = src[:, :, 0:half]
        x2 = src[:, :, half:2 * half]
        cb = cos.unsqueeze(1).to_broadcast([128, H, half])
        sn = sin.unsqueeze(1).to_broadcast([128, H, half])
        t = [tmp[:, k, 0:H * half].rearrange("p (h c) -> p h c", c=half) for k in range(4)]
        self.tt(eng, t[0], x1, cb, ALU.mult, reads, [t_tmp])
        self.tt(eng, t[1], x2, sn, ALU.mult, reads, [t_tmp])
        self.tt(eng, t[2], x2, cb, ALU.mult, reads, [t_tmp])
        self.tt(eng, t[3], x1, sn, ALU.mult, reads, [t_tmp])
        self.tt(eng, dst[:, :, 0:half], t[0], t[1], ALU.subtract, [t_tmp], [t_dst])
        self.tt(eng, dst[:, :, half:2 * half], t[2], t[3], ALU.add, [t_tmp], [t_dst])

    def phase_b(self):
        S, i, s = self.S, self.i, self.s
        NOWN = self.NOWN
        with ExitStack() as st:
            w_own = self.sb(st, "w_own", [128, KC, 3088], BF16)
            w_kv = self.sb(st, "w_kv", [128, KC, 576], BF16)
            t_wown, t_wkv = S.tok("w_own"), S.tok("w_kv")
            wv = i["w_in"].rearrange("(k p) n -> p k n", p=128)
            t_wparts = S.toks("wpart", 6)
            self.ld(w_kv[:, :, 0:512], wv[:, :, C_K:C_K + 512], [t_wkv], t_wkv, eng="pool")
            self.ld(w_kv[:, :, 512:576], wv[:, :, C_KI:C_KI + 64], [t_wkv], t_wkv, eng="pool")
            self.ld(w_own[:, :, 0:1024], wv[:, :, C_Q:C_Q + 1024], [t_wown], t_wown, eng="pool")
            self.ld(w_own[:, :, 1024:2048], wv[:, :, C_QI:C_QI + 1024], [t_wown], t_wown, eng="pool")
            self.ld(w_own[:, :, 2048:2064], wv[:, :, C_WI:C_WI + 16], [t_wown], t_wown, eng="pool")
            self.ld(w_own[:, :, 2064:3088], wv[:, :, C_P:C_P + 1024], [t_wown], t_wown, eng="pool")
            wpool = self.sb(st, "wpool", [128, 4, 2, 256], BF16)
            t_wpool = S.tok("wpool")
            self.ld(wpool[:], i["w_pool"].rearrange("g (c p) d -> p g c d", p=128), [t_wpool], t_wpool, eng="pool")
            pscol = self.sb(st, "pscol", [128, 8], F32)
            t_ps = S.tok("pscol")
            self.ld(pscol[:], i["pscol"], [t_ps], t_ps)
            poolA = self.sb(st, "poolA", [128, 12 + 4 * NOWN, 128], BF16)
            t_pA = S.tok("poolA")
            self.ld(poolA[:], i["poolA"], [t_pA], t_pA)
            rrep = self.sb(st, "rrep", [16, 128], BF16)
            maski = self.sb(st, "maski", [128, 16, 128], BF16)
            t_rr, t_mi = S.tok("rrep"), S.tok("maski")
            self.ld(rrep[:], i["rrep"], [t_rr], t_rr)
            self.ld(maski[:], i["maski"], [t_mi], t_mi)
            qg = self.sb(st, "qg", [128, 128], F32)
            kg = self.sb(st, "kg", [128, 128], F32)
            t_qg, t_kg = S.tok("qg"), S.tok("kg")
            self.ld(qg[:], i["qg"].partition_broadcast(128), [t_qg], t_qg)
            self.ld(kg[:], i["kg"].partition_broadcast(128), [t_kg], t_kg)
            self.ts("dve", qg[:], qg[:], float(HD ** -0.5), None, ALU.mult, None, [t_qg], [t_qg])
            epsc = self.sb(st, "epsc", [128, 1], F32)
            t_eps = S.tok("epsc")
            S.op("dve", lambda e: e.memset(epsc[:], EPS), writes=[t_eps])

            xt = [self.sb(st, "xt%d" % k, [128, D], F32) for k in range(2)]
            t_xt = S.toks("xt", 2)
            R = {
                "ss": self.sb(st, "ss", [128, 4], F32), "t_ss": S.tok("ss"),
                "xs": self.sb(st, "xs", [128, D], F32), "t_xs": S.tok("xs"),
                "xtp": self.ps(st, "xtp", [128, D], F32), "t_xtp": S.toks("xtp", 4),
                "epsc": epsc, "t_eps": t_eps,
            }
            R["junk"], R["t_junk"] = R["xs"], R["t_xs"]
            hT = [self.sb(st, "hT%d" % k, [128, KC, 128], BF16) for k in range(2)]
            t_hT = S.toks("hT", 2)
            rp = [self.sb(st, "rp%d" % k, [128, 48], F32) for k in range(2)]
            t_rp = S.toks("rp", 2)
            pj = [self.ps(st, "pj%d" % k, [128, 512], F32) for k in range(2)]
            t_pj = S.toks("pj", 2)
            tq = self.ps(st, "tq", [128, 1024], BF16)
            t_tq = S.tok("tq")
            pm = self.ps(st, "pm", [128, 512], F32)
            t_pm = S.tok("pm")
            hss = self.sb(st, "hss", [128, 16], F32)
            t_hss = S.tok("hss")
            kn = self.sb(st, "kn", [128, NG, 128], F32)
            knb = self.sb(st, "knb", [128, NG, 128], BF16)
            t_kn, t_knb = S.tok("kn"), S.tok("knb")
            qn = self.sb(st, "qn", [128, NH, 128], F32)
            qnb = self.sb(st, "qnb", [128, NH, 128], BF16)
            t_qn, t_qnb = S.tok("qn"), S.tok("qnb")
            qif = qn[:].rearrange("p h d -> p (h d)").rearrange("p (h d) -> p h d", d=IDM)
            qib_t = self.sb(st, "qib", [128, IH, IDM], BF16)
            qib = qib_t[:]
            t_qif, t_qib = t_qn, S.tok("qib")
            kif = self.sb(st, "kif", [128, 1, IDM], F32)
            kib = self.sb(st, "kib", [128, 2, IDM], BF16)
            t_kif, t_kib = S.tok("kif"), S.tok("kib")
            rtmp = self.sb(st, "rtmp", [128, 4, 128], F32)
            t_rtmp = S.tok("rtmp")
            vb = self.sb(st, "vb", [128, 256], BF16)
            t_vb = S.tok("vb")
            kst = self.sb(st, "kst", [128, NG, 128], BF16)
            t_kst = S.tok("kst")
            kist = self.sb(st, "kist", [128, 128], BF16)
            t_kist = S.tok("kist")
            qst = self.sb(st, "qst", [128, NH, 128], BF16)
            t_qst = S.tok("qst")
            qist = self.sb(st, "qist", [128, 16, 128], BF16)
            t_qist = S.tok("qist")
            S.op("pool", lambda e: e.memset(qist[:], 0.0), writes=[t_qist])
            wib = self.sb(st, "wib", [128, 16], BF16)
            wT = self.sb(st, "wT", [16, 128], BF16)
            t_wib, t_wT = S.tok("wib"), S.tok("wT")
            wbd = self.sb(st, "wbd", [128, 16, 128], BF16)
            t_wbd = S.tok("wbd")
            pcur = [self.sb(st, "pcur%d" % k, [128, PW], BF16) for k in range(2)]
            t_pcur = S.toks("pcur", 2)
            phalo = self.sb(st, "phalo", [128, PW], BF16)
            t_phalo = S.tok("phalo")
            mixsb = self.sb(st, "mixsb", [128, 8, 128], BF16)
            t_mixsb = S.tok("mixsb")
            mixst = self.sb(st, "mixst", [128, 8, 128], BF16)
            t_mixst = S.tok("mixst")
            SH1 = self.cols[:, 0:16]

            tails = []

            def proj(dst_ps, t_dst, h, t_h, w, t_w, c0, n):
                for kc in range(KC):
                    self.mm(dst_ps[:, 0:n], h[:, kc, :], w[:, kc, c0:c0 + n], kc == 0, kc == KC - 1, [t_h, t_w], [t_dst])

            def head_norm(ps_ap, H, gain, t_gain, dst, t_dst, t_ps, col0):
                for hh in range(H):
                    self.act(R["junk"][:, hh * 128:(hh + 1) * 128], ps_ap[:, hh * 128:(hh + 1) * 128], AF.Square,
                             [t_ps], [R["t_junk"], t_hss], accum_out=hss[:, hh:hh + 1])
                self.act(hss[:, 4:4 + H], hss[:, 0:H], AF.Sqrt, [t_hss, t_eps], [t_hss], bias=epsc[:, 0:1], scale=1.0 / HD)
                S.op("dve", lambda e: e.reciprocal(out=hss[:, 8:8 + H], in_=hss[:, 4:4 + H]), reads=[t_hss], writes=[t_hss])
                for hh in range(H):
                    self.stt("dve", dst[:, col0 + hh, :], ps_ap[:, hh * 128:(hh + 1) * 128], hss[:, 8 + hh:9 + hh], gain[:],
                             ALU.mult, ALU.mult, [t_ps, t_hss, t_gain], [t_dst])

            def kv_part(blk, b, rpb, t_rpb):
                tok0 = blk * 128
                A, B = pj[0], pj[1]
                proj(A, t_pj[0], hT[b], t_hT[b], w_kv, t_wkv, 0, 512)
                proj(B, t_pj[1], hT[b], t_hT[b], w_kv, t_wkv, 512, 64)
                head_norm(A[:, 0:256], NG, kg, t_kg, kn, t_kn, t_pj[0], 0)
                self.cp("act", vb[:], A[:, 256:512], [t_pj[0]], [t_vb])
                self.cp("act", kif[:, 0, :], B[:, 0:64], [t_pj[1]], [t_kif])
                self.cp("act", knb[:], kn[:], [t_kn], [t_knb])
                self.rope_ops(kn, knb, NG, 16, rpb[:, 0:16], rpb[:, 16:32], rtmp, [t_kn, t_rpb], t_knb, t_rtmp)
                self.cp("act", kib[:, 0, :], kif[:, 0, :], [t_kif], [t_kib])
                self.rope_ops(kif, kib[:, 0:1, :], 1, 8, rpb[:, 32:40], rpb[:, 40:48], rtmp, [t_kif, t_rpb], t_kib, t_rtmp)
                self.cp("act", kib[:, 1, :], kib[:, 0, :], [t_kib], [t_kib])
                self.stg(s["v"][tok0:tok0 + 128, :], vb[:], [t_vb], t_vb, writes=[self.st_tok["v"]])

                def kv_tail():
                    for g in range(NG):
                        self.tr(tq[:, g * 128:(g + 1) * 128], knb[:, g, :], self.ident_b[:], [t_knb, self.t_const], [t_tq])
                    self.tr(tq[:, 256:384], kib[:].rearrange("p a d -> p (a d)"), self.ident_b[:], [t_kib, self.t_const], [t_tq])
                    self.cp("dve", kst[:].rearrange("p g t -> p (g t)"), tq[:, 0:256], [t_tq], [t_kst])
                    self.cp("dve", kist[:], tq[:, 256:384], [t_tq], [t_kist])
                    self.stg(s["kT"][:, :, tok0:tok0 + 128], kst[:], [t_kst], t_kst, writes=[self.st_tok["kT"]])
                    self.stg(s["kiT"][:, tok0:tok0 + 128], kist[:], [t_kist], t_kist, writes=[self.st_tok["kiT"]])
                tails.append(kv_tail)

            def pool_proj(b, dst, t_dst):
                for half in range(2):
                    P_ = pj[half]
                    proj(P_, t_pj[half], hT[b], t_hT[b], w_own, t_wown, 2064 + half * 512, 512)
                    self.cp("act", dst[:, half * 512:(half + 1) * 512], P_[:], [t_pj[half]], [t_dst])

            def own_part(ob, b, rpb, t_rpb, pc, t_pc, pprev, t_pprev, a_cur, a_prev):
                for half in range(2):
                    P_ = pj[half]
                    proj(P_, t_pj[half], hT[b], t_hT[b], w_own, t_wown, half * 512, 512)
                    head_norm(P_[:], 4, qg, t_qg, qn, t_qn, t_pj[half], half * 4)
                self.cp("act", qnb[:], qn[:], [t_qn], [t_qnb])
                self.rope_ops(qn, qnb, NH, 16, rpb[:, 0:16], rpb[:, 16:32], rtmp, [t_qn, t_rpb], t_qnb, t_rtmp)
                def q_tail():
                    for hh in range(NH):
                        self.tr(tq[:, hh * 128:(hh + 1) * 128], qnb[:, hh, :], self.ident_b[:], [t_qnb, self.t_const], [t_tq])
                    self.cp("dve", qst[:].rearrange("p h t -> p (h t)"), tq[:], [t_tq], [t_qst])
                    self.stg(s["qT"][ob], qst[:], [t_qst], t_qst, writes=[self.st_tok["qT"]])
                tails.append(q_tail)
                for half in range(2):
                    P_ = pj[half]
                    proj(P_, t_pj[half], hT[b], t_hT[b], w_own, t_wown, 1024 + half * 512, 512)
                    self.cp("act", qif[:, half * 8:(half + 1) * 8, :].rearrange("p h d -> p (h d)"), P_[:], [t_pj[half]], [t_qif])
                self.cp("act", qib[:], qif[:], [t_qif], [t_qib])
                self.rope_ops(qif, qib, IH, 8, rpb[:, 32:40], rpb[:, 40:48], rtmp, [t_qif, t_rpb], t_qib, t_rtmp)
                def qi_tail():
                    for c in range(8):
                        self.tr(tq[:, c * 128:(c + 1) * 128], qib[:, 2 * c:2 * c + 2, :].rearrange("p h d -> p (h d)"), self.ident_b[:],
                                [t_qib, self.t_const], [t_tq])
                    for e_ in range(2):
                        self.cp("dve" if e_ == 0 else "act",
                                qist[e_ * 64:(e_ + 1) * 64, :, e_ * 64:(e_ + 1) * 64].rearrange("p g (c t) -> p c g t", c=8),
                                tq[e_ * 64:(e_ + 1) * 64, :].rearrange("p (c g t) -> p c g t", c=8, g=16), [t_tq], [t_qist])
                    self.stg(s["qiT"][ob], qist[:], [t_qist], t_qist, writes=[self.st_tok["qiT"]])
                tails.append(qi_tail)
                P_ = pj[0]
                proj(P_, t_pj[0], hT[b], t_hT[b], w_own, t_wown, 2048, 16)
                self.cp("act", wib[:], P_[:, 0:16], [t_pj[0]], [t_wib])
                self.tr(tq[0:16, 0:128], wib[:], self.ident_b[:], [t_wib, self.t_const], [t_tq])
                self.cp("dve", wT[:], tq[0:16, 0:128], [t_tq], [t_wT])
                P_ = pj[1]
                self.mm(P_[:, 0:128], rrep[:], wT[:], True, True, [t_rr, t_wT], [t_pj[1]])
                self.tt("dve", wbd[:], P_[:, 0:128].unsqueeze(1).to_broadcast([128, 16, 128]), maski[:], ALU.mult,
                        [t_pj[1], t_mi], [t_wbd])
                self.stg(s["wbd"][ob], wbd[:], [t_wbd], t_wbd, writes=[self.st_tok["wbd"]])
                pool_proj(b, pc, t_pc)
                for gp in range(2):
                    for gg in range(2):
                        g = gp * 2 + gg
                        for cc in range(2):
                            o_ = pm[:, (gg * 2 + cc) * 128:(gg * 2 + cc + 1) * 128]
                            ch = slice(g * 256 + cc * 128, g * 256 + (cc + 1) * 128)
                            self.mm(o_, pc[:, ch], poolA[:, a_cur + g, :], True, False, [t_pc, t_pA], [t_pm])
                            self.mm(o_, pprev[:, ch], poolA[:, a_prev + g, :], False, True, [t_pprev, t_pA], [t_pm])
                    self.cp("act", mixsb[:, gp * 4:(gp + 1) * 4, :].rearrange("p a t -> p (a t)"), pm[:], [t_pm], [t_mixsb])
                for g in range(4):
                    P_ = pj[g % 2]
                    for dc in range(2):
                        for cc in range(2):
                            self.mm(P_[:, dc * 128:(dc + 1) * 128], wpool[:, g, cc, dc * 128:(dc + 1) * 128], mixsb[:, g * 2 + cc, :],
                                    cc == 0, cc == 1, [t_wpool, t_mixsb], [t_pj[g % 2]])
                    for dc in range(2):
                        self.ts("dve", mixst[:, g * 2 + dc, :], P_[:, dc * 128:(dc + 1) * 128], pscol[:, g * 2 + dc:g * 2 + dc + 1], None,
                                ALU.mult, None, [t_pj[g % 2], t_ps], [t_mixst])
                self.stg(s["mixT"][ob, :, 8:16, :], mixst[:], [t_mixst], t_mixst, writes=[self.st_tok["mixT"]])

            self.ld(xt[0][:], i["x_halo"], [t_xt[0]], t_xt[0])
            self.norm_block(xt[0], t_xt[0], self.G1, SH1, hT[0], t_hT[0], R)
            pool_proj(0, phalo, t_phalo)

            def norm_stage(blk):
                b = blk % 2
                self.ld(xt[b][:], i["x_loc"][blk * 128:(blk + 1) * 128, :], [t_xt[b]], t_xt[b])
                self.ld(rp[b][:], i["rope"][blk * 128:(blk + 1) * 128, :], [t_rp[b]], t_rp[b])
                self.norm_block(xt[b], t_xt[b], self.G1, SH1, hT[b], t_hT[b], R)

            def proj_stage(blk):
                b = blk % 2
                del tails[:]
                chunk = blk // 4
                own = (chunk % 2 == 0)
                kv_part(blk, b, rp[b], t_rp[b])
                if own:
                    j = chunk // 2
                    bi = blk % 4
                    ob = j * 4 + bi
                    pb = ob % 2
                    if bi == 0:
                        a_cur = 8 if j == 0 else 0
                        own_part(ob, b, rp[b], t_rp[b], pcur[pb], t_pcur[pb], phalo, t_phalo, a_cur, 12 + 4 * j)
                    else:
                        own_part(ob, b, rp[b], t_rp[b], pcur[pb], t_pcur[pb], pcur[1 - pb], t_pcur[1 - pb], 0, 4)
                for fn_ in tails:
                    fn_()

            norm_stage(0)
            for blk in range(self.NBA):
                if blk + 1 < self.NBA:
                    norm_stage(blk + 1)
                proj_stage(blk)

    def phase_c(self):
        S, i, s = self.S, self.i, self.s
        SEQ = self.SEQ
        NKB = SEQ // 128
        NB = N_BISECT
        with ExitStack() as st:
            kiT = self.sb(st, "kiT", [128, SEQ], BF16)
            t_kiT = S.tok("kiT")
            self.ld(kiT[:], s["kiT"], [t_kiT], t_kiT, reads=[self.st_tok["kiT"]])
            maskadd = self.sb(st, "maskadd", [128, 4, 1024], BF16)
            t_ma = S.tok("maskadd")
            self.ld(maskadd[:], i["maskadd"], [t_ma], t_ma)
            score = [self.sb(st, "score%d" % k, [128, SEQ], F32) for k in range(2)]
            t_score = S.toks("score", 2)
            m01 = [self.sb(st, "m01%d" % k, [128, SEQ], BF16) for k in range(2)]
            t_m01 = S.toks("m01", 2)
            mT = self.sb(st, "mT", [128, NKB, 128], BF16)
            t_mT = S.tok("mT")
            NQ = 3
            qTb = [self.sb(st, "qTb%d" % k, [128, NH, 128], BF16) for k in range(NQ)]
            t_qTb = S.toks("qTb", NQ)
            qiTb = [self.sb(st, "qiTb%d" % k, [128, 16, 128], BF16) for k in range(2)]
            wbdb = [self.sb(st, "wbdb%d" % k, [128, 16, 128], BF16) for k in range(2)]
            t_qiTb, t_wbdb = S.toks("qiTb", 2), S.toks("wbdb", 2)
            NRL = 6
            rl = [self.sb(st, "rl%d" % k, [128, 512], BF16) for k in range(NRL)]
            t_rl = S.toks("rl", NRL)
            NPT = 6
            pT = [self.sb(st, "pT%d" % k, [128, 512], BF16) for k in range(NPT)]
            t_pT = S.toks("pT", NPT)
            NKV = 3
            kTc = [self.sb(st, "kTc%d" % k, [128, NG, 512], BF16) for k in range(NKV)]
            Vc = [self.sb(st, "Vc%d" % k, [128, 4, 256], BF16) for k in range(NKV)]
            t_kTc, t_Vc = S.toks("kTc", NKV), S.toks("Vc", NKV)
            bis = [self.sb(st, "bis%d" % k, [128, 16], F32) for k in range(2)]
            t_bis = S.toks("bis", 2)
            dtab = [self.sb(st, "dtab%d" % k, [128, NB], F32) for k in range(2)]
            dtab2 = [self.sb(st, "dtab2%d" % k, [128, NB], F32) for k in range(2)]
            t_dtab = S.toks("dtab", 2)
            pow2 = self.sb(st, "pow2", [128, NB], F32)
            t_pow2 = S.tok("pow2")
            for k in range(NB):
                S.op("pool", lambda e, k=k: e.memset(pow2[:, k:k + 1], float(2.0 ** -(k + 2))), writes=[t_pow2])
            tmpf = self.sb(st, "tmpf", [128, 512], F32)
            t_tmpf = S.tok("tmpf")
            ast = [self.sb(st, "ast%d" % k, [128, NH, 128], BF16) for k in range(2)]
            t_ast = S.toks("ast", 2)
            rec = [self.sb(st, "rec%d" % k, [128, 512], F32) for k in range(2)]
            t_rec = S.toks("rec", 2)
            NW = 5
            pw = [self.ps(st, "pw%d" % k, [128, 512], F32) for k in range(NW)]
            t_pw = S.toks("pw", NW)
            sc = self.ps(st, "sc", [128, 512], F32)
            t_sc = S.tok("sc")
            og1 = self.ps(st, "og", [128, 512], F32)
            sm1 = self.ps(st, "sm", [128, 512], F32)
            t_og1, t_sm1 = S.tok("og"), S.tok("sm")
            DI, DA = 3, 2
            cnt = {"w": 0, "r": 0, "p": 0, "kv": 0}
            vkb = s["v"].rearrange("(n p) c -> p n c", p=128)

            def qinfo(n):
                j, bi = n // 4, n % 4
                return j, bi, (2 * j + 2), (2 * j + 2) * 512

            def gen_I(n):
                j, bi, nkc, nk = qinfo(n)
                b = n % 2
                self.ld(qiTb[b][:], s["qiT"][n], [t_qiTb[b]], t_qiTb[b], reads=[self.st_tok["qiT"]])
                self.ld(wbdb[b][:], s["wbd"][n], [t_wbdb[b]], t_wbdb[b], reads=[self.st_tok["wbd"]])
                items = [(kc, gg, 0) for kc in range(nkc) for gg in range(16)]
                slot = {}
                for idx in range(len(items) + DI):
                    if idx < len(items):
                        kc, ii, e_ = items[idx]
                        w = cnt["w"] % NW
                        cnt["w"] += 1
                        r = cnt["r"] % NRL
                        cnt["r"] += 1
                        slot[idx] = r
                        self.mm(pw[w][:], qiTb[b][:, ii, :], kiT[:, kc * 512:(kc + 1) * 512], True, True,
                                [t_qiTb[b], t_kiT], [t_pw[w]])
                        self.act(rl[r][:], pw[w][:], AF.Relu, [t_pw[w]], [t_rl[r]])
                    k2 = idx - DI
                    if k2 >= 0:
                        kc, ii, e_ = items[k2]
                        r = slot.pop(k2)
                        n_in = ii
                        self.mm(sc[:], wbdb[b][:, ii, :], rl[r][:], n_in == 0, n_in == 15, [t_wbdb[b], t_rl[r]], [t_sc])
                        if n_in == 15:
                            dst = score[b][:, kc * 512:(kc + 1) * 512]
                            if kc >= 2 * j:
                                mk = maskadd[:, bi, (kc - 2 * j) * 512:(kc - 2 * j + 1) * 512]
                                self.tt("dve", dst, sc[:], mk, ALU.add, [t_sc, t_ma], [t_score[b]])
                                self.tt("dve", tmpf[:], sc[:], mk, ALU.subtract, [t_sc, t_ma], [t_tmpf])
                                S.op("dve", lambda e, c=3 + (kc - 2 * j), b=b: e.tensor_reduce(out=bis[b][:, c:c + 1], in_=tmpf[:], axis=AX.X, op=ALU.min),
                                     reads=[t_tmpf], writes=[t_bis[b]])
                            else:
                                self.cp("act", dst, sc[:], [t_sc], [t_score[b]])
                    if idx % 2 == 1:
                        yield

            def gen_B(n):
                j, bi, nkc, nk = qinfo(n)
                b = n % 2
                q3 = n % NQ
                B_, tb = bis[b], t_bis[b]
                self.ld(qTb[q3][:], s["qT"][n], [t_qTb[q3]], t_qTb[q3], reads=[self.st_tok["qT"]])
                S.op("dve", lambda e: e.tensor_reduce(out=B_[:, 0:1], in_=score[b][:, 0:nk], axis=AX.X, op=ALU.max),
                     reads=[t_score[b]], writes=[tb])
                if j > 0:
                    S.op("dve", lambda e: e.tensor_reduce(out=B_[:, 2:3], in_=score[b][:, 0:2 * j * 512], axis=AX.X, op=ALU.min),
                         reads=[t_score[b]], writes=[tb])
                    S.op("dve", lambda e: e.tensor_reduce(out=B_[:, 1:2], in_=B_[:, 2:5], axis=AX.X, op=ALU.min), reads=[tb], writes=[tb])
                else:
                    S.op("dve", lambda e: e.tensor_reduce(out=B_[:, 1:2], in_=B_[:, 3:5], axis=AX.X, op=ALU.min), reads=[tb], writes=[tb])
                yield
                self.tt("dve", B_[:, 5:6], B_[:, 0:1], B_[:, 1:2], ALU.subtract, [tb], [tb])
                self.ts("dve", B_[:, 5:6], B_[:, 5:6], 1.02, 2e-6, ALU.mult, ALU.add, [tb], [tb])
                self.tt("dve", B_[:, 6:7], B_[:, 0:1], B_[:, 1:2], ALU.add, [tb], [tb])
                self.ts("dve", B_[:, 6:7], B_[:, 6:7], 0.5, None, ALU.mult, None, [tb], [tb])
                self.ts("dve", dtab[b][:], pow2[:], B_[:, 5:6], None, ALU.mult, None, [tb, t_pow2], [t_dtab[b]])
                self.ts("dve", dtab2[b][:], dtab[b][:], 2.0, None, ALU.mult, None, [t_dtab[b]], [t_dtab[b]])
                yield
                for it in range(NB):
                    self.ts("dve", m01[b][:, 0:nk], score[b][:, 0:nk], B_[:, 6:7], None, ALU.is_ge, ALU.add, [t_score[b], tb], [t_m01[b], tb],
                            accum_out=B_[:, 7:8])
                    self.ts("dve", B_[:, 8:9], B_[:, 7:8], float(TOPK), dtab2[b][:, it:it + 1], ALU.is_ge, ALU.mult, [tb, t_dtab[b]], [tb])
                    self.ts("dve", B_[:, 6:7], B_[:, 8:9], dtab[b][:, it:it + 1], B_[:, 6:7], ALU.subtract, ALU.add, [tb, t_dtab[b]], [tb])
                    yield
                self.ts("dve", B_[:, 9:10], B_[:, 6:7], dtab[b][:, NB - 1:NB], None, ALU.subtract, None, [tb, t_dtab[b]], [tb])
                self.ts("dve", m01[b][:, 0:nk], score[b][:, 0:nk], B_[:, 9:10], None, ALU.is_ge, None, [t_score[b], tb], [t_m01[b]])

            def gen_A(n):
                j, bi, nkc, nk = qinfo(n)
                b = n % 2
                q3 = n % NQ
                for k4 in range(nk // 512):
                    w = cnt["w"] % NW
                    cnt["w"] += 1
                    tv = pw[w].bitcast(BF16)
                    for q_ in range(4):
                        kb = k4 * 4 + q_
                        self.tr(tv[:, q_ * 128:(q_ + 1) * 128], m01[b][:, kb * 128:(kb + 1) * 128], self.ident_b[:],
                                [t_m01[b], self.t_const], [t_pw[w]])
                    self.cp("act", mT[:, k4 * 4:(k4 + 1) * 4, :].rearrange("p a q -> p (a q)"), tv[:, 0:512], [t_pw[w]], [t_mT])
                    if k4 % 2 == 1:
                        yield
                nkb = nk // 128
                for g in range(NG):
                    items = [(k4, q_) for k4 in range(nk // 512) for q_ in range(4)]
                    slot = {}
                    kvof = {}
                    for idx in range(len(items) + DA):
                        if idx < len(items):
                            k4, q_ = items[idx]
                            if q_ == 0:
                                kv = cnt["kv"] % NKV
                                cnt["kv"] += 1
                                kvof[k4] = kv
                                self.ld(kTc[kv][:, 0, :], s["kT"][:, g, k4 * 512:(k4 + 1) * 512], [t_kTc[kv]], t_kTc[kv], reads=[self.st_tok["kT"]])
                                self.ld(Vc[kv][:, :, 0:128], vkb[:, k4 * 4:(k4 + 1) * 4, g * 128:(g + 1) * 128], [t_Vc[kv]], t_Vc[kv], reads=[self.st_tok["v"]])
                            kv = kvof[k4]
                            kb = k4 * 4 + q_
                            w = cnt["w"] % NW
                            cnt["w"] += 1
                            pp = cnt["p"] % NPT
                            cnt["p"] += 1
                            slot[idx] = pp
                            self.mm(pw[w][:], kTc[kv][:, 0, q_ * 128:(q_ + 1) * 128], qTb[q3][:, g * 4:(g + 1) * 4, :], True, True,
                                    [t_kTc[kv], t_qTb[q3]], [t_pw[w]])
                            self.act(pT[pp][:], pw[w][:], AF.Exp, [t_pw[w]], [t_pT[pp]])
                            pv = pT[pp][:].rearrange("p (h q) -> p h q", h=4)
                            self.tt("pool", pv, pv, mT[:, kb, :].unsqueeze(1).to_broadcast([128, 4, 128]), ALU.mult, [t_pT[pp], t_mT], [t_pT[pp]])
                        k2 = idx - DA
                        if k2 >= 0:
                            k4, q_ = items[k2]
                            kv = kvof[k4]
                            kb = k4 * 4 + q_
                            pp = slot.pop(k2)
                            self.mm(og1[:], Vc[kv][:, q_, 0:128], pT[pp][:], kb == 0, kb == nkb - 1, [t_Vc[kv], t_pT[pp]], [t_og1])
                            self.mm(sm1[:], self.ones_b[:], pT[pp][:], kb == 0, kb == nkb - 1, [self.t_const, t_pT[pp]], [t_sm1])
                        yield
                    S.op("dve", lambda e, g=g: e.reciprocal(out=rec[g][:], in_=sm1[:]), reads=[t_sm1], writes=[t_rec[g]])
                    self.tt("dve", ast[b][:, g * 4:(g + 1) * 4, :].rearrange("p h q -> p (h q)"), og1[:], rec[g][:], ALU.mult,
                            [t_og1, t_rec[g]], [t_ast[b]])
                self.stg(s["mixT"][n, :, 0:8, :], ast[b][:], [t_ast[b]], t_ast[b], writes=[self.st_tok["mixT"]])

            def units(kind, n):
                j, bi, nkc, nk = qinfo(n)
                return {"I": nkc * 8 + 2, "B": NB + 2, "A": 2 * (nkc * 4 + DA) + max(1, nkc // 2)}[kind] + 1

            def run_parallel(gens):
                state = [[g, float(u), 0] for g, u in gens]
                while state:
                    state.sort(key=lambda x: x[2] / x[1])
                    g = state[0]
                    try:
                        next(g[0])
                        g[2] += 1
                    except StopIteration:
                        state.pop(0)

            NQB = self.NBO
            for step in range(NQB + 2):
                gens = []
                if step - 2 >= 0:
                    gens.append((gen_A(step - 2), units("A", step - 2)))
                if 0 <= step - 1 < NQB:
                    gens.append((gen_B(step - 1), units("B", step - 1)))
                if step < NQB:
                    gens.append((gen_I(step), units("I", step)))
                run_parallel(gens)

    def phase_d(self):
        S, i, s = self.S, self.i, self.s
        with ExitStack() as st:
            w_out = self.sb(st, "w_out", [128, KC, D], BF16)
            t_wout = S.toks("w_out", 4)
            wv = i["w_out"].rearrange("(k p) n -> p k n", p=128)
            for nb in range(4):
                self.ld(w_out[:, :, nb * 512:(nb + 1) * 512], wv[:, :, nb * 512:(nb + 1) * 512], [t_wout[nb]], t_wout[nb], eng="pool")
            w_rt = self.sb(st, "w_rt", [128, KC, 36], F32)
            t_wrt = S.tok("w_rt")
            self.ld(w_rt[:], i["w_rt"].rearrange("(k p) n -> p k n", p=128), [t_wrt], t_wrt)
            gt1 = self.sb(st, "gt1", [128, D], F32)
            t_gt1 = S.tok("gt1")
            self.ld(gt1[:], s["gt"][0:1, :].partition_broadcast(128), [t_gt1], t_gt1, reads=[self.st_tok["gt"]])
            epsc = self.sb(st, "epsc", [128, 1], F32)
            t_eps = S.tok("epsc")
            S.op("dve", lambda e: e.memset(epsc[:], EPS), writes=[t_eps])
            xt = [self.sb(st, "xt%d" % k, [128, D], F32) for k in range(2)]
            t_xt = S.toks("xt", 2)
            mx = [self.sb(st, "mx%d" % k, [128, KC, 128], BF16) for k in range(2)]
            t_mx = S.toks("mx", 2)
            R = {
                "ss": self.sb(st, "ss", [128, 4], F32), "t_ss": S.tok("ss"),
                "xs": self.sb(st, "xs", [128, D], F32), "t_xs": S.tok("xs"),
                "xtp": self.ps(st, "xtp", [128, D], F32), "t_xtp": S.toks("xtp", 4),
                "epsc": epsc, "t_eps": t_eps,
            }
            R["junk"], R["t_junk"] = R["xs"], R["t_xs"]
            hT = [self.sb(st, "hT%d" % k, [128, KC, 128], BF16) for k in range(2)]
            t_hT = S.toks("hT", 2)
            hTf = self.sb(st, "hTf", [128, KC, 128], F32)
            t_hTf = S.tok("hTf")
            tmp = [self.sb(st, "tmp%d" % k, [128, 512], F32) for k in range(2)]
            t_tmp = S.toks("tmp", 2)
            pj = [self.ps(st, "pj%d" % k, [128, 512], F32) for k in range(2)]
            t_pj = S.toks("pj", 2)
            prt = self.ps(st, "prt", [128, 512], F32)
            t_prt = S.tok("prt")
            rt = self.sb(st, "rt", [128, 36], F32)
            t_rt = S.tok("rt")
            rs = self.sb(st, "rs", [128, 64], F32)
            t_rs = S.tok("rs")
            SH2 = self.cols[:, 48:64]
            ncnt = [0]

            def wout_stage(ob):
                j, bi = ob // 4, ob % 4
                b = ob % 2
                tok0 = (2 * j) * 512 + bi * 128
                self.ld(xt[b][:], i["x_loc"][tok0:tok0 + 128, :], [t_xt[b]], t_xt[b])
                self.ld(mx[b][:], s["mixT"][ob], [t_mx[b]], t_mx[b], reads=[self.st_tok["mixT"]])
                for nb in range(4):
                    w = ncnt[0] % 2
                    ncnt[0] += 1
                    for kc in range(KC):
                        self.mm(pj[w][:], mx[b][:, kc, :], w_out[:, kc, nb * 512:(nb + 1) * 512], kc == 0, kc == KC - 1,
                                [t_mx[b], t_wout[nb]], [t_pj[w]])
                    self.tt("dve", tmp[w][:], pj[w][:], gt1[:, nb * 512:(nb + 1) * 512], ALU.mult, [t_pj[w], t_gt1], [t_tmp[w]])
                    self.tt("pool", xt[b][:, nb * 512:(nb + 1) * 512], xt[b][:, nb * 512:(nb + 1) * 512], tmp[w][:], ALU.add,
                            [t_tmp[w], t_xt[b]], [t_xt[b]])
                self.stg(s["x1"][ob * 128:(ob + 1) * 128, :], xt[b][:], [t_xt[b]], t_xt[b], writes=[self.st_tok["x1"]])

            def post_stage(ob):
                b = ob % 2
                self.norm_block(xt[b], t_xt[b], self.G2, SH2, hT[b], t_hT[b], R, hTf=hTf, t_hTf=t_hTf)
                self.stg(s["h2T"][ob], hT[b][:], [t_hT[b]], t_hT[b], writes=[self.st_tok["h2T"]])
                for kc in range(KC):
                    self.mm(prt[:, 0:36], hTf[:, kc, :], w_rt[:, kc, :], kc == 0, kc == KC - 1, [t_hTf, t_wrt], [t_prt])
                self.cp("act", rt[:], prt[:, 0:36], [t_prt], [t_rt])
                dv = lambda *a, **k: None
                X = lambda a, b_: rs[:, a:b_]
                S.op("dve", lambda e: e.tensor_reduce(out=X(0, 1), in_=rt[:, 0:4], axis=AX.X, op=ALU.max), reads=[t_rt], writes=[t_rs])
                self.ts("dve", X(4, 8), rt[:, 0:4], X(0, 1), None, ALU.is_equal, None, [t_rt, t_rs], [t_rs])
                self.ts("dve", X(1, 2), X(0, 1), -1.0, None, ALU.mult, None, [t_rs], [t_rs])
                self.act(X(8, 12), rt[:, 0:4], AF.Exp, [t_rt, t_rs], [t_rs], bias=X(1, 2), accum_out=X(2, 3))
                S.op("dve", lambda e: e.reciprocal(out=X(3, 4), in_=X(2, 3)), reads=[t_rs], writes=[t_rs])
                self.ts("dve", X(16, 24), rt[:, 4:12], X(4, 5), None, ALU.mult, None, [t_rt, t_rs], [t_rs])
                for g in range(1, 4):
                    self.stt("dve", X(16, 24), rt[:, 4 + 8 * g:12 + 8 * g], X(4 + g, 5 + g), X(16, 24), ALU.mult, ALU.add, [t_rt, t_rs], [t_rs])
                S.op("dve", lambda e: e.tensor_reduce(out=X(12, 13), in_=X(16, 24), axis=AX.X, op=ALU.max), reads=[t_rs], writes=[t_rs])
                self.ts("dve", X(24, 32), X(16, 24), X(12, 13), None, ALU.is_equal, None, [t_rs], [t_rs])
                self.stt("dve", X(32, 40), X(24, 32), NEG, X(16, 24), ALU.mult, ALU.add, [t_rs], [t_rs])
                S.op("dve", lambda e: e.tensor_reduce(out=X(13, 14), in_=X(32, 40), axis=AX.X, op=ALU.max), reads=[t_rs], writes=[t_rs])
                self.ts("dve", X(40, 48), X(32, 40), X(13, 14), None, ALU.is_equal, None, [t_rs], [t_rs])
                self.tt("dve", X(14, 15), X(13, 14), X(12, 13), ALU.subtract, [t_rs], [t_rs])
                self.act(X(15, 16), X(14, 15), AF.Exp, [t_rs], [t_rs])
                self.ts("dve", X(15, 16), X(15, 16), 1.0, None, ALU.add, None, [t_rs], [t_rs])
                S.op("dve", lambda e: e.reciprocal(out=X(48, 49), in_=X(15, 16)), reads=[t_rs], writes=[t_rs])
                self.tt("dve", X(49, 50), X(48, 49), X(3, 4), ALU.mult, [t_rs], [t_rs])
                self.tt("dve", X(50, 51), X(3, 4), X(49, 50), ALU.subtract, [t_rs], [t_rs])
                self.ts("dve", X(52, 60), X(24, 32), X(49, 50), None, ALU.mult, None, [t_rs], [t_rs])
                self.stt("dve", X(52, 60), X(40, 48), X(50, 51), X(52, 60), ALU.mult, ALU.add, [t_rs], [t_rs])
                for g in range(4):
                    self.ts("dve", self.comb[:, ob, g * 8:(g + 1) * 8], X(52, 60), X(4 + g, 5 + g), None, ALU.mult, None, [t_rs], [self.t_comb])

            wout_stage(0)
            for ob in range(self.NBO):
                if ob + 1 < self.NBO:
                    wout_stage(ob + 1)
                post_stage(ob)

    def phase_e(self):
        S, i, s = self.S, self.i, self.s
        with ExitStack() as st:
            gt2 = self.sb(st, "gt2", [128, D], F32)
            t_gt2 = S.tok("gt2")
            self.ld(gt2[:], s["gt"][1:2, :].partition_broadcast(128), [t_gt2], t_gt2, reads=[self.st_tok["gt"]])
            h2 = self.sb(st, "h2", [128, KC, 512], BF16)
            t_h2 = S.tok("h2")
            t_h2p = S.toks("h2p", 4)
            yacc = self.sb(st, "yacc", [128, 4, D], F32)
            t_yacc = S.toks("yacc", 4)
            w1b = [self.sb(st, "w1b%d" % k, [128, KC, DEXP], BF16) for k in range(2)]
            w3b = [self.sb(st, "w3b%d" % k, [128, KC, DEXP], BF16) for k in range(2)]
            w2b = [self.sb(st, "w2b%d" % k, [128, 4, D], BF16) for k in range(2)]
            t_w1, t_w3, t_w2 = S.toks("w1b", 2), S.toks("w3b", 2), S.toks("w2b", 2)
            aT = self.sb(st, "aT", [128, 4, 512], BF16)
            t_aT = S.toks("aT", 4)
            s1 = [self.sb(st, "s1%d" % k, [128, 512], BF16) for k in range(2)]
            t_s1 = S.toks("s1", 2)
            x1t = [self.sb(st, "x1t%d" % k, [128, D], F32) for k in range(2)]
            t_x1t = S.toks("x1t", 2)
            ph1 = [self.ps(st, "ph1%d" % k, [128, 512], F32) for k in range(2)]
            ph3 = [self.ps(st, "ph3%d" % k, [128, 512], F32) for k in range(2)]
            py = [self.ps(st, "py%d" % k, [128, 512], F32) for k in range(2)]
            t_ph1, t_ph3, t_py = S.toks("ph1", 2), S.toks("ph3", 2), S.toks("py", 2)
            n_h = 0
            n_y = 0
            n_e = 0
            n_x = 0
            for j in range(self.NOWN):
                for bi in range(4):
                    self.ld(h2[:, :, bi * 128:(bi + 1) * 128], s["h2T"][4 * j + bi], [t_h2], t_h2, reads=[self.st_tok["h2T"]])
                for e_ in range(NEXP):
                    wb = n_e % 2
                    n_e += 1
                    self.ld(w1b[wb][:], i["w1"][e_].rearrange("(k p) f -> p k f", p=128), [t_w1[wb]], t_w1[wb], eng="pool")
                    self.ld(w3b[wb][:], i["w3"][e_].rearrange("(k p) f -> p k f", p=128), [t_w3[wb]], t_w3[wb], eng="pool")
                    self.ld(w2b[wb][:], i["w2"][e_].rearrange("(k p) n -> p k n", p=128), [t_w2[wb]], t_w2[wb], eng="pool")
                    for fc in range(4):
                        hb = n_h % 2
                        n_h += 1
                        for kc in range(KC):
                            self.mm(ph1[hb][:], w1b[wb][:, kc, fc * 128:(fc + 1) * 128], h2[:, kc, :], kc == 0, kc == KC - 1,
                                    [t_w1[wb], t_h2], [t_ph1[hb]])
                        for kc in range(KC):
                            self.mm(ph3[hb][:], w3b[wb][:, kc, fc * 128:(fc + 1) * 128], h2[:, kc, :], kc == 0, kc == KC - 1,
                                    [t_w3[wb], t_h2], [t_ph3[hb]])
                        self.act(s1[hb][:], ph1[hb][:], AF.Silu, [t_ph1[hb]], [t_s1[hb]])
                        self.tt("dve", aT[:, fc, :], s1[hb][:], ph3[hb][:], ALU.mult, [t_s1[hb], t_ph3[hb]], [t_aT[fc]])
                    for bi in range(4):
                        cw = self.comb[:, 4 * j + bi, e_:e_ + 1]
                        for nb in range(4):
                            yb = n_y % 2
                            n_y += 1
                            for fc in range(4):
                                self.mm(py[yb][:], aT[:, fc, bi * 128:(bi + 1) * 128], w2b[wb][:, fc, nb * 512:(nb + 1) * 512], fc == 0, fc == 3,
                                        [t_aT[fc], t_w2[wb]], [t_py[yb]])
                            ys = yacc[:, bi, nb * 512:(nb + 1) * 512]
                            if e_ == 0:
                                self.ts("dve", ys, py[yb][:], cw, None, ALU.mult, None, [t_py[yb], self.t_comb], [t_yacc[bi]])
                            else:
                                self.stt("dve", ys, py[yb][:], cw, ys, ALU.mult, ALU.add, [t_py[yb], self.t_comb, t_yacc[bi]], [t_yacc[bi]])
                for bi in range(4):
                    xb = n_x % 2
                    n_x += 1
                    ob = 4 * j + bi
                    self.ld(x1t[xb][:], s["x1"][ob * 128:(ob + 1) * 128, :], [t_x1t[xb]], t_x1t[xb], reads=[self.st_tok["x1"]])
                    self.tt("pool", yacc[:, bi, :], yacc[:, bi, :], gt2[:], ALU.mult, [t_yacc[bi], t_gt2], [t_yacc[bi]])
                    self.tt("pool", x1t[xb][:], x1t[xb][:], yacc[:, bi, :], ALU.add, [t_x1t[xb], t_yacc[bi]], [t_x1t[xb]])
                    self.stg(self.out[ob * 128:(ob + 1) * 128, :], x1t[xb][:], [t_x1t[xb]], t_x1t[xb])


def _bf(a):
    return np.ascontiguousarray(a).astype(ml_dtypes.bfloat16)


def _rope_table(pos):
    pos = pos.astype(np.float32)
    out = np.zeros((pos.shape[0], 48), np.float32)
    for (rd, c0) in ((32, 0), (16, 32)):
        half = rd // 2
        inv = np.float32(500000.0) ** (-(np.arange(half, dtype=np.float32) * np.float32(2.0)) / np.float32(rd))
        ang = (pos[:, None] * inv[None, :]).astype(np.float32)
        out[:, c0:c0 + half] = np.cos(ang)
        out[:, c0 + half:c0 + 2 * half] = np.sin(ang)
    return out


def _pool_mats(NOWN, p):
    wins = (2, 4, 8, 16)
    M = np.zeros((128, 12 + 4 * NOWN, 128), np.float32)
    src = np.arange(128)[:, None]
    dst = np.arange(128)[None, :]
    for g, w in enumerate(wins):
        cur = ((src <= dst) & (src >= dst - w + 1)).astype(np.float32) / w - (src == dst)
        prev = ((src - 128) >= (dst - w + 1)).astype(np.float32) / w
        M[:, g, :] = cur
        M[:, 4 + g, :] = prev
        if p == 0:
            cnt = np.minimum(dst + 1, w).astype(np.float32)
            M[:, 8 + g, :] = ((src <= dst) & (src >= dst - w + 1)).astype(np.float32) / cnt - (src == dst)
        else:
            M[:, 8 + g, :] = cur
        for j in range(NOWN):
            if p == 0 and j == 0:
                continue
            r = src - 16 * j
            valid = (r >= 0) & (r < 16) & ((r - 16) >= (dst - w + 1))
            M[:, 12 + 4 * j + g, :] = valid.astype(np.float32) / w
    return _bf(M)


def _consts(p, NOWN):
    c = {}
    c["ident_f"] = np.eye(128, dtype=np.float32)
    c["ident_b"] = _bf(np.eye(128, dtype=np.float32))
    rr = np.zeros((16, 128), np.float32)
    mi = np.zeros((128, 16, 128), np.float32)
    for e in range(2):
        for cc in range(8):
            for t in range(8):
                r = e * 64 + cc * 8 + t
                rr[2 * cc + e, r] = 1.0
                for gg in range(16):
                    mi[r, gg, 8 * gg + t] = 1.0
    c["rrep"] = _bf(rr)
    c["maski"] = _bf(mi)
    ma = np.zeros((128, 4, 1024), np.float32)
    kk = np.arange(512)[None, :]
    for bi in range(4):
        qq = bi * 128 + np.arange(128)[:, None]
        ma[:, bi, 0:512] = np.where(kk <= qq, 0.0, NEG)
        ma[:, bi, 512:1024] = 0.0 if p == 1 else NEG
    c["maskadd"] = _bf(ma)
    c["poolA"] = _pool_mats(NOWN, p)
    return c


def _col(v, n):
    return np.ascontiguousarray(np.asarray(v, np.float32).reshape(n, 128).T)


def make_in_maps(inputs):
    x = np.asarray(inputs["x"], np.float32)
    B, SEQ, _ = x.shape
    NL = SEQ // 512
    NOWN = NL // 2
    shared = {
        "w_ada": np.ascontiguousarray(inputs["w_ada"][0], dtype=np.float32),
        "b_ada": np.ascontiguousarray(inputs["b_ada"][0], dtype=np.float32).reshape(1, -1),
        "g1col": _col(inputs["norm1_g"][0], KC),
        "g2col": _col(inputs["norm2_g"][0], KC),
        "w_in": np.ascontiguousarray(inputs["w_in"][0], dtype=np.float32),
        "qg": np.asarray(inputs["q_norm_g"][0], np.float32).reshape(1, 128),
        "kg": np.asarray(inputs["k_norm_g"][0], np.float32).reshape(1, 128),
        "w_pool": np.ascontiguousarray(inputs["w_pool"][0], dtype=np.float32),
        "pscol": _col(np.asarray(inputs["pool_scale"][0]).reshape(-1), 8),
        "w_out": np.ascontiguousarray(inputs["w_out"][0], dtype=np.float32),
        "w_rt": np.ascontiguousarray(np.concatenate([inputs["w_grp"][0], inputs["w_exp"][0]], axis=1), dtype=np.float32),
        "w1": np.ascontiguousarray(inputs["w1"][0], dtype=np.float32),
        "w3": np.ascontiguousarray(inputs["w3"][0], dtype=np.float32),
        "w2": np.ascontiguousarray(inputs["w2"][0], dtype=np.float32),
    }
    consts = [_consts(p, NOWN) for p in range(2)]
    maps = []
    for b in range(B):
        for p in range(2):
            order = [(s ^ 1) if p == 1 else s for s in range(NL)]
            xl = np.concatenate([x[b, g * 512:(g + 1) * 512] for g in order], axis=0)
            pos = np.concatenate([np.arange(g * 512, (g + 1) * 512) for g in order])
            halo = np.zeros((128, D), np.float32)
            for j in range(NOWN):
                g = order[2 * j]
                if g > 0:
                    halo[16 * j:16 * j + 16] = x[b, g * 512 - 16:g * 512]
            m = dict(shared)
            m.update(consts[p])
            m["x_loc"] = np.ascontiguousarray(xl)
            m["x_halo"] = halo
            m["cvec"] = _col(inputs["c"][b], KC)
            m["rope"] = _rope_table(pos)
            maps.append(m)
    return maps, B, SEQ


_CACHE = {}


def kernel(**inputs):
    maps, B, SEQ = make_in_maps(inputs)
    NL = SEQ // 512
    if SEQ not in _CACHE:
        _CACHE[SEQ] = Prog(SEQ).build()
    nc = _CACHE[SEQ]
    res = run_bass_kernel_spmd(nc, maps, core_ids=list(range(2 * B)))
    out = np.zeros((B, SEQ, D), np.float32)
    for b in range(B):
        for p in range(2):
            o = res.results[2 * b + p]["out"]
            for j in range(NL // 2):
                g = 2 * j + p
                out[b, g * 512:(g + 1) * 512] = o[j * 512:(j + 1) * 512]
    return out
```
